# Optimizing a Trainium2 kernel written in Bass

```python
import jax
import jax.numpy as jnp
from jax import lax
import numpy as np

D_MODEL = 1024
BATCH = 4
SEQ = 8192
DEPTH = 2

HEAD_DIM = 64
BLOCK = 128
SWA_Q_HEADS = 8
SWA_KV_HEADS = 2
SWA_GROUP = SWA_Q_HEADS // SWA_KV_HEADS
WINDOW = 128
SWA_WIDTH = SWA_Q_HEADS * HEAD_DIM
KV_WIDTH = SWA_KV_HEADS * HEAD_DIM
LRU_WIDTH = D_MODEL // 4
LRU_BLOCKS = 8
LRU_BLOCK_W = LRU_WIDTH // LRU_BLOCKS
CONV_WIDTH = 4
LRU_C = 8.0
FOX_HEADS = 4
FOX_WIDTH = FOX_HEADS * HEAD_DIM
D_MIX = SWA_WIDTH + LRU_WIDTH + FOX_WIDTH
OFF_KA = SWA_WIDTH
OFF_VA = OFF_KA + KV_WIDTH
OFF_XB = OFF_VA + KV_WIDTH
OFF_GB = OFF_XB + LRU_WIDTH
OFF_QC = OFF_GB + LRU_WIDTH
OFF_KC = OFF_QC + FOX_WIDTH
OFF_VC = OFF_KC + FOX_WIDTH
OFF_FC = OFF_VC + FOX_WIDTH
IN_COLS = OFF_FC + FOX_HEADS
IN_SPLITS = (OFF_KA, OFF_VA, OFF_XB, OFF_GB, OFF_QC, OFF_KC, OFF_VC, OFF_FC)
N_GROUPS = 4
EXPERTS_PER_GROUP = 4
N_EXPERTS = N_GROUPS * EXPERTS_PER_GROUP
TOP_K = 2
D_EXPERT = 256
EPS = 1e-6
F32 = jnp.float32

kernel_name = 'hymba_style_hybrid_swa_rglru_fox_hmoe_adaln'


def rms_normalize(x):
    xf = x.astype(F32)
    return xf * lax.rsqrt(jnp.mean(xf * xf, axis=-1, keepdims=True) + EPS)


def rms_norm(x, gain):
    return (rms_normalize(x) * gain.astype(F32)).astype(x.dtype)


def modulate(h, shift, scale):
    return h * (1.0 + scale[:, None, :]) + shift[:, None, :]


def alibi_slopes(n_heads):
    return jnp.exp2(-8.0 * jnp.arange(1, n_heads + 1, dtype=F32) / n_heads)


def sliding_window_sink_attention(q, k, v, sinks):
    b, s, _, dh = q.shape
    nb = s // BLOCK
    qb = q.reshape(b, nb, BLOCK, SWA_KV_HEADS, SWA_GROUP, dh)

    def band(t):
        tb = t.reshape(b, nb, BLOCK, SWA_KV_HEADS, dh)
        prev = jnp.pad(tb[:, :-1], ((0, 0), (1, 0), (0, 0), (0, 0), (0, 0)))
        return jnp.concatenate([prev, tb], axis=2)

    kb, vb = band(k), band(v)
    scores = jnp.einsum('bnqhgd,bnkhd->bnhgqk', qb, kb).astype(F32) * (dh ** -0.5)
    q_pos = BLOCK + jnp.arange(BLOCK)
    k_pos = jnp.arange(2 * BLOCK)
    dist = q_pos[:, None] - k_pos[None, :]
    in_window = (dist >= 0) & (dist < WINDOW)
    block_exists = (jnp.arange(nb)[:, None, None] > 0) | (k_pos[None, None, :] >= BLOCK)
    valid = in_window[None] & block_exists
    slopes = alibi_slopes(SWA_Q_HEADS).reshape(SWA_KV_HEADS, SWA_GROUP)
    scores = scores - slopes[:, :, None, None] * dist.astype(F32)
    scores = jnp.where(valid[None, :, None, None], scores, -jnp.inf)
    sink = jnp.broadcast_to(sinks.astype(F32).reshape(SWA_KV_HEADS, SWA_GROUP, 1, 1),
                            scores.shape[:-1] + (1,))
    probs = jax.nn.softmax(jnp.concatenate([scores, sink], axis=-1), axis=-1)[..., :-1]
    out = jnp.einsum('bnhgqk,bnkhd->bnqhgd', probs.astype(v.dtype), vb)
    return out.reshape(b, s, SWA_Q_HEADS * dh)


def causal_depthwise_conv(x, w, bias):
    s = x.shape[1]
    xp = jnp.pad(x, ((0, 0), (CONV_WIDTH - 1, 0), (0, 0)))
    y = bias
    for k in range(CONV_WIDTH):
        y = y + xp[:, k:k + s] * w[k]
    return y


def rg_lru(x, w_a, b_a, w_x, b_x, lam):
    b, s, w = x.shape
    xb = x.reshape(b, s, LRU_BLOCKS, LRU_BLOCK_W)
    gate_a = jnp.einsum('bsnc,ncd->bsnd', xb, w_a).reshape(b, s, w) + b_a
    gate_x = jnp.einsum('bsnc,ncd->bsnd', xb, w_x).reshape(b, s, w) + b_x
    r = jax.nn.sigmoid(gate_a.astype(F32))
    i = jax.nn.sigmoid(gate_x.astype(F32))
    log_a = -LRU_C * r * jax.nn.softplus(-lam.astype(F32))
    a = jnp.exp(log_a)
    u = jnp.sqrt(-jnp.expm1(2.0 * log_a)) * (i * x.astype(F32))

    def combine(left, right):
        a_l, u_l = left
        a_r, u_r = right
        return a_l * a_r, a_r * u_l + u_r

    _, hs = lax.associative_scan(combine, (a, u), axis=1)
    return hs.astype(x.dtype)


def forgetting_attention(q, k, v, log_f):
    b, s, h, dh = q.shape
    nb = s // BLOCK
    cum = jnp.cumsum(log_f, axis=1).transpose(0, 2, 1)
    q_blocks = q.reshape(b, nb, BLOCK, h, dh).transpose(1, 0, 2, 3, 4)
    cum_q = cum.reshape(b, h, nb, BLOCK).transpose(2, 0, 1, 3)
    k_pos = jnp.arange(s)

    def attend_block(args):
        n, q_n, cq_n = args
        logits = jnp.einsum('bqhd,bkhd->bhqk', q_n, k).astype(F32) * (dh ** -0.5)
        logits = logits + (cq_n[..., :, None] - cum[:, :, None, :])
        q_pos = n * BLOCK + jnp.arange(BLOCK)
        causal = k_pos[None, :] <= q_pos[:, None]
        logits = jnp.where(causal, logits, -jnp.inf)
        probs = jax.nn.softmax(logits, axis=-1)
        return jnp.einsum('bhqk,bkhd->bqhd', probs.astype(v.dtype), v)

    out = lax.map(attend_block, (jnp.arange(nb), q_blocks, cum_q))
    return out.transpose(1, 0, 2, 3, 4).reshape(b, s, h * dh)


def hybrid_mixer(h, w_in, w_out, out_gain, sinks, conv_w, conv_b,
                 lru_wa, lru_ba, lru_wx, lru_bx, lru_lam, fox_bf):
    b, s, _ = h.shape
    proj = jnp.einsum('bsd,de->bse', h, w_in)
    q_a, k_a, v_a, x_b, g_b, q_c, k_c, v_c, f_c = jnp.split(proj, IN_SPLITS, axis=-1)
    y_a = sliding_window_sink_attention(
        q_a.reshape(b, s, SWA_Q_HEADS, HEAD_DIM),
        k_a.reshape(b, s, SWA_KV_HEADS, HEAD_DIM),
        v_a.reshape(b, s, SWA_KV_HEADS, HEAD_DIM), sinks)
    x_conv = causal_depthwise_conv(x_b, conv_w, conv_b)
    y_b = rg_lru(x_conv, lru_wa, lru_ba, lru_wx, lru_bx, lru_lam) * jax.nn.gelu(g_b)
    log_f = jax.nn.log_sigmoid(f_c.astype(F32) + fox_bf.astype(F32))
    y_c = forgetting_attention(
        q_c.reshape(b, s, FOX_HEADS, HEAD_DIM),
        k_c.reshape(b, s, FOX_HEADS, HEAD_DIM),
        v_c.reshape(b, s, FOX_HEADS, HEAD_DIM), log_f)
    y = jnp.concatenate([rms_normalize(y_a), rms_normalize(y_b), rms_normalize(y_c)], axis=-1)
    y = (y * out_gain.astype(F32)).astype(h.dtype)
    return jnp.einsum('bse,ed->bsd', y, w_out)


def hierarchical_moe(h, w_rg, b_rg, w_re, b_re, w_gate, w_up, w_down):
    b, s, _ = h.shape
    group_logits = (jnp.einsum('bsd,dg->bsg', h, w_rg) + b_rg).astype(F32)
    group_prob = jax.nn.softmax(group_logits, axis=-1)
    group_p, group_idx = lax.top_k(group_prob, 1)
    expert_logits = (jnp.einsum('bsd,de->bse', h, w_re) + b_re).astype(F32)
    expert_logits = expert_logits.reshape(b, s, N_GROUPS, EXPERTS_PER_GROUP)
    in_group = jnp.take_along_axis(expert_logits, group_idx[..., None], axis=2)[:, :, 0]
    expert_prob = jax.nn.softmax(in_group, axis=-1)
    top_p, top_local = lax.top_k(expert_prob, TOP_K)
    weights = group_p * top_p / jnp.sum(top_p, axis=-1, keepdims=True)
    expert_idx = group_idx * EXPERTS_PER_GROUP + top_local
    combine = jnp.sum(jax.nn.one_hot(expert_idx, N_EXPERTS, dtype=F32) * weights[..., None], axis=-2)
    combine = combine.astype(h.dtype)
    y = jnp.zeros_like(h)
    for e in range(N_EXPERTS):
        hidden = jax.nn.silu(h @ w_gate[e]) * (h @ w_up[e])
        y = y + combine[..., e:e + 1] * (hidden @ w_down[e])
    return y


def setup_inputs(seed: int = 0) -> dict:
    key = jax.random.key(seed)
    ks = jax.random.split(key, 26)
    L, D = DEPTH, D_MODEL

    def nrm(k, shape, scale):
        return scale * jax.random.normal(k, shape, F32)

    gate_offset = jnp.concatenate([jnp.zeros((2 * D,), F32), jnp.ones((D,), F32),
                                   jnp.zeros((2 * D,), F32), jnp.ones((D,), F32)])
    u = jax.random.uniform(ks[16], (L, LRU_WIDTH), F32, 0.9, 0.999)
    root = u ** (1.0 / LRU_C)
    lru_lam = jnp.log(root) - jnp.log1p(-root)
    return {
        'x': nrm(ks[0], (BATCH, SEQ, D), 1.0),
        'c': nrm(ks[1], (BATCH, D), 1.0),
        'w_mod': nrm(ks[2], (L, D, 6 * D), 0.25 * D ** -0.5),
        'b_mod': nrm(ks[3], (L, 6 * D), 0.1) + gate_offset,
        'norm_mix': 1.0 + nrm(ks[4], (L, D), 0.1),
        'norm_ffn': 1.0 + nrm(ks[5], (L, D), 0.1),
        'w_in': nrm(ks[6], (L, D, IN_COLS), D ** -0.5),
        'w_out': nrm(ks[7], (L, D_MIX, D), D_MIX ** -0.5),
        'out_gain': 1.0 + nrm(ks[8], (L, D_MIX), 0.1),
        'sinks': nrm(ks[9], (L, SWA_Q_HEADS), 0.5),
        'conv_w': nrm(ks[10], (L, CONV_WIDTH, LRU_WIDTH), CONV_WIDTH ** -0.5),
        'conv_b': nrm(ks[11], (L, LRU_WIDTH), 0.02),
        'lru_wa': nrm(ks[12], (L, LRU_BLOCKS, LRU_BLOCK_W, LRU_BLOCK_W), LRU_BLOCK_W ** -0.5),
        'lru_ba': nrm(ks[13], (L, LRU_WIDTH), 0.02),
        'lru_wx': nrm(ks[14], (L, LRU_BLOCKS, LRU_BLOCK_W, LRU_BLOCK_W), LRU_BLOCK_W ** -0.5),
        'lru_bx': nrm(ks[15], (L, LRU_WIDTH), 0.02),
        'lru_lam': lru_lam,
        'fox_bf': jax.random.uniform(ks[17], (L, FOX_HEADS), F32, 1.0, 5.0),
        'w_router_group': nrm(ks[18], (L, D, N_GROUPS), D ** -0.5),
        'b_router_group': nrm(ks[19], (L, N_GROUPS), 0.01),
        'w_router_expert': nrm(ks[20], (L, D, N_EXPERTS), D ** -0.5),
        'b_router_expert': nrm(ks[21], (L, N_EXPERTS), 0.01),
        'w_gate': nrm(ks[22], (L, N_EXPERTS, D, D_EXPERT), D ** -0.5),
        'w_up': nrm(ks[23], (L, N_EXPERTS, D, D_EXPERT), D ** -0.5),
        'w_down': nrm(ks[24], (L, N_EXPERTS, D_EXPERT, D), D_EXPERT ** -0.5),
        'norm_final': 1.0 + nrm(ks[25], (D,), 0.1),
    }


def reference(x, c, w_mod, b_mod, norm_mix, norm_ffn, w_in, w_out, out_gain, sinks,
              conv_w, conv_b, lru_wa, lru_ba, lru_wx, lru_bx, lru_lam, fox_bf,
              w_router_group, b_router_group, w_router_expert, b_router_expert,
              w_gate, w_up, w_down, norm_final):
    c_act = jax.nn.silu(c)
    for l in range(DEPTH):
        mod = c_act @ w_mod[l] + b_mod[l]
        shift_m, scale_m, gate_m, shift_f, scale_f, gate_f = jnp.split(mod, 6, axis=-1)
        h = modulate(rms_norm(x, norm_mix[l]), shift_m, scale_m)
        x = x + gate_m[:, None, :] * hybrid_mixer(
            h, w_in[l], w_out[l], out_gain[l], sinks[l], conv_w[l], conv_b[l],
            lru_wa[l], lru_ba[l], lru_wx[l], lru_bx[l], lru_lam[l], fox_bf[l])
        h = modulate(rms_norm(x, norm_ffn[l]), shift_f, scale_f)
        x = x + gate_f[:, None, :] * hierarchical_moe(
            h, w_router_group[l], b_router_group[l], w_router_expert[l], b_router_expert[l],
            w_gate[l], w_up[l], w_down[l])
    return rms_norm(x, norm_final)
```

```python
import numpy as np
from contextlib import ExitStack
import concourse.bass as bass
import concourse.mybir as mybir
from concourse.bass_utils import run_bass_kernel_spmd

F32 = mybir.dt.float32
BF16 = mybir.dt.bfloat16
AF = mybir.ActivationFunctionType
ALU = mybir.AluOpType
AX = mybir.AxisListType

D = 1024
L_FULL = 2
T_FULL = 4096
IN_COLS = 2052
EPS = 1e-6
NEG = -30000.0
COMPUTE = ('pe', 'act', 'dve', 'pool')
ENGS = ('pe', 'act', 'dve', 'pool', 'sp')
BLKNAME = {'pe': 'tensor', 'act': 'scalar', 'dve': 'vector', 'pool': 'gpsimd', 'sp': 'sync'}


class Res:
    __slots__ = ('name', 'lw', 'rd', 'sem', 'semcnt')

    def __init__(self, name):
        self.name = name
        self.lw = None
        self.rd = {}
        self.sem = None
        self.semcnt = 0


class Sched:
    BLK = 8192
    NS = 4

    def __init__(self, nc, es):
        self.nc = nc
        self.es = es
        self.q = {e: [] for e in ENGS}
        self.cnt = {e: 0 for e in COMPUTE}
        self.esem = {e: [es.enter_context(nc.semaphore(f"s_{e}{i}")) for i in range(self.NS)] for e in COMPUTE}
        self.waited = {e: {} for e in ENGS}
        self.pending = {e: None for e in ENGS}
        self.dma_res = []
        self.nsem = 0
        self.nops = 0
        self.rescache = {}

    def res(self, name):
        if name not in self.rescache:
            self.rescache[name] = Res(name)
        return self.rescache[name]

    def _semval(self, src, idx):
        s = self.esem[src][(idx // self.BLK) % self.NS]
        v = (idx // (self.BLK * self.NS)) * self.BLK + (idx % self.BLK) + 1
        return s, v

    def op(self, eng, fn, reads=(), writes=(), dma=None, inc=16):
        deps = set()
        for r in reads:
            if r.lw is not None:
                deps.add(r.lw)
        for w in writes:
            if w.lw is not None:
                deps.add(w.lw)
            for h in w.rd.values():
                deps.add(h)
        if self.pending[eng] is not None:
            deps |= self.pending[eng]
            self.pending[eng] = None
        if dma is not None:
            if dma.sem is None:
                dma.sem = self.es.enter_context(self.nc.semaphore(f"d_{self.nsem}"))
                self.nsem += 1
                self.dma_res.append(dma)
            dma.semcnt += inc
            h = ('d', dma, dma.semcnt, inc)
        else:
            assert eng in COMPUTE
            h = ('c', eng, self.cnt[eng])
            self.cnt[eng] += 1
        self.q[eng].append((fn, deps, h))
        self.nops += 1
        for r in reads:
            key = h[1]
            r.rd[key] = h
        for w in writes:
            w.lw = h
            w.rd = {}
        return h

    def barrier(self):
        hs = set()
        for e in COMPUTE:
            if self.cnt[e] > 0:
                hs.add(('c', e, self.cnt[e] - 1))
        for r in self.dma_res:
            if r.semcnt > 0:
                hs.add(('d', r, r.semcnt, 0))
        for e in ENGS:
            self.pending[e] = set(hs) if self.pending[e] is None else (self.pending[e] | hs)

    def _emit(self, ename, eng, ops):
        waited = self.waited[ename]
        for fn, deps, h in ops:
            for d in deps:
                if d[0] == 'c':
                    _, src, idx = d
                    if src == ename and ename == 'pe':
                        continue
                    if waited.get(src, -1) >= idx:
                        continue
                    waited[src] = idx
                    sm, v = self._semval(src, idx)
                    eng.wait_ge(sm, v)
                else:
                    r, c = d[1], d[2]
                    if waited.get(r, -1) >= c:
                        continue
                    waited[r] = c
                    eng.wait_ge(r.sem, c)
            ins = fn(eng)
            if h[0] == 'c':
                sm, v = self._semval(h[1], h[2])
                ins.then_inc(sm, 1)
            elif h[3] == 16:
                ins.then_inc(h[1].sem, 16)
            else:
                ins.then_inc(h[1].sem)

    def flush(self):
        if not any(self.q[e] for e in ENGS):
            return
        with self.nc.Block() as blk:
            for e in ENGS:
                ops = self.q[e]
                if not ops:
                    continue

                def body(eng, e=e, ops=ops):
                    self._emit(e, eng, ops)
                getattr(blk, BLKNAME[e])(body)
        for e in ENGS:
            self.q[e] = []

    def finish(self):
        self.barrier()
        deps = self.pending['sp']
        self.pending['sp'] = None
        with self.nc.Block() as blk:
            def body(eng):
                for d in deps:
                    if d[0] == 'c':
                        sm, v = self._semval(d[1], d[2])
                        eng.wait_ge(sm, v)
                    else:
                        eng.wait_ge(d[1].sem, d[2])
            blk.sync(body)


class K:
    def __init__(self, T=T_FULL, L=L_FULL, dbg=False, stop_after=None):
        self.T, self.L, self.dbg = T, L, dbg
        self.stop_after = stop_after
        self.NT = T // 128
        self.NCH = T // 512
        self.rr = 0

    def un(self, name):
        self.uid = getattr(self, 'uid', 0) + 1
        return f"{name}_{self.uid}"

    def dma(self, q, out, in_, reads=(), writes=(), owner=None, **kw):
        return self.s.op(q, lambda e: e.dma_start(out=out, in_=in_, **kw), reads, writes, dma=owner)

    def allgather(self, src, dst, name):
        R = self.s.res(name)
        self.s.barrier()
        h = self.s.op('pool', lambda e: e.collective_compute("AllGather", ALU.bypass,
                                                            replica_groups=[[0, 1], [2, 3], [4, 5], [6, 7]],
                                                            ins=[src.opt()], outs=[dst.opt()]), (), (), dma=R, inc=1)
        return h

    def mm(self, out, lhsT, rhs, start, stop, reads, writes):
        return self.s.op('pe', lambda e: e.matmul(out, lhsT, rhs, start=start, stop=stop), reads, writes)

    def tr(self, out, in_, ident, reads, writes):
        return self.s.op('pe', lambda e: e.transpose(out, in_, ident), reads, writes)

    def act(self, out, in_, func, reads, writes, bias=None, scale=None, accum_out=None, eng='act'):
        kw = {}
        if bias is not None:
            kw['bias'] = bias
        if scale is not None:
            kw['scale'] = scale
        if accum_out is not None:
            kw['accum_out'] = accum_out
        return self.s.op('act', lambda e: e.activation(out=out, in_=in_, func=func, **kw), reads, writes)

    def ts(self, eng, out, in0, s1, s2, op0, op1, reads, writes, accum_out=None):
        if op1 is None:
            return self.s.op(eng, lambda e: e.tensor_scalar(out=out, in0=in0, scalar1=s1, scalar2=None, op0=op0), reads, writes)
        return self.s.op(eng, lambda e: e.tensor_scalar(out=out, in0=in0, scalar1=s1, scalar2=s2, op0=op0, op1=op1), reads, writes)

    def tt(self, eng, out, in0, in1, op, reads, writes):
        return self.s.op(eng, lambda e: e.tensor_tensor(out=out, in0=in0, in1=in1, op=op), reads, writes)

    def stt(self, out, in0, scalar, in1, op0, op1, reads, writes):
        return self.s.op('dve', lambda e: e.scalar_tensor_tensor(out=out, in0=in0, scalar=scalar, in1=in1, op0=op0, op1=op1), reads, writes)

    def cp(self, eng, out, in_, reads, writes):
        if eng == 'act':
            return self.s.op('act', lambda e: e.copy(out=out, in_=in_), reads, writes)
        return self.s.op(eng, lambda e: e.tensor_copy(out=out, in_=in_), reads, writes)

    def memset(self, eng, ap, val, writes):
        return self.s.op(eng, lambda e: e.memset(ap, val), (), writes)

    def recip(self, out, in_, reads, writes):
        return self.s.op('dve', lambda e: e.reciprocal(out=out, in_=in_), reads, writes)

    def scan(self, out, d0, d1, init, op0, op1, reads, writes):
        return self.s.op('dve', lambda e: e.tensor_tensor_scan(out=out, data0=d0, data1=d1, initial=init, op0=op0, op1=op1), reads, writes)

    def asel(self, out, in_, pattern, cmp, fill, base, cm, reads, writes):
        return self.s.op('pool', lambda e: e.affine_select(out=out, in_=in_, pattern=pattern, compare_op=cmp, fill=fill, base=base, channel_multiplier=cm), reads, writes)

    def evac_eng(self):
        self.rr += 1
        return 'act' if self.rr % 2 else 'dve'

    def evac(self, out, in_, reads, writes, scale=None, eng=None):
        eng = eng or self.evac_eng()
        if eng == 'act':
            if scale is None:
                return self.s.op('act', lambda e: e.copy(out=out, in_=in_), reads, writes)
            return self.s.op('act', lambda e: e.activation(out=out, in_=in_, func=AF.Copy, scale=scale), reads, writes)
        if scale is None:
            return self.s.op('dve', lambda e: e.tensor_copy(out=out, in_=in_), reads, writes)
        return self.s.op('dve', lambda e: e.tensor_scalar(out=out, in0=in_, scalar1=scale, scalar2=None, op0=ALU.mult), reads, writes)

    def build(self):
        T, L = self.T, self.L
        nc = bass.Bass("TRN2", target_bir_lowering=False)
        self.nc = nc
        dt = nc.dram_tensor

        def inp(name, shape, dtype=F32):
            return dt(name, list(shape), dtype, kind="ExternalInput").ap()
        self.x_in = inp("x", [T, D])
        self.cT = inp("cT", [128, 8])
        self.w_mod = inp("w_mod", [L_FULL, D, 6 * D])
        self.b_mod = inp("b_mod", [L_FULL, 6 * D])
        self.norm_mix = inp("norm_mix", [L_FULL, D])
        self.norm_ffn = inp("norm_ffn", [L_FULL, D])
        self.w_in = inp("w_in", [L_FULL, D, IN_COLS])
        self.w_out = inp("w_out", [L_FULL, D, D])
        self.gainT = inp("gainT", [L_FULL, 128, 8])
        self.sinks = inp("sinks", [L_FULL, 8])
        self.convT = inp("convT", [L_FULL, 128, 2, 4])
        self.convb = inp("convb", [L_FULL, 128, 2])
        self.lru_wa = inp("lru_wa", [L_FULL, 8, 32, 32])
        self.lru_ba = inp("lru_ba", [L_FULL, 128, 2])
        self.lru_wx = inp("lru_wx", [L_FULL, 8, 32, 32])
        self.lru_bx = inp("lru_bx", [L_FULL, 128, 2])
        self.lru_lam = inp("lru_lam", [L_FULL, 128, 2])
        self.fox_bf = inp("fox_bf", [L_FULL, 4])
        self.w_rg = inp("w_rg", [L_FULL, D, 4])
        self.b_rg = inp("b_rg", [L_FULL, 4])
        self.w_re = inp("w_re", [L_FULL, D, 16])
        self.b_re = inp("b_re", [L_FULL, 16])
        self.w_gate = inp("w_gate", [L_FULL, 16, D, 256])
        self.w_up = inp("w_up", [L_FULL, 16, D, 256])
        self.w_down = inp("w_down", [L_FULL, 16, 256, D])
        self.norm_final = inp("norm_final", [D])
        self.flags = inp("flags", [128, 4])
        self.out = dt("out", [T, D], F32, kind="ExternalOutput").ap()
        sk = "ExternalOutput" if self.dbg else "Internal"

        def scr(name, shape, dtype):
            kind = "Internal" if name in ("KcT", "Vc", "EXC", "EXS", "EXH", "EXCg", "EXSg", "EXHg", "KcTg", "Vcg") else sk
            return dt(name, list(shape), dtype, kind=kind).ap()
        self.xs = scr("xs", [T, D], F32)
        self.QaT = scr("QaT", [512, T], BF16)
        self.KaT = scr("KaT", [128, T], BF16)
        self.Va = scr("Va", [T, 128], BF16)
        self.QcT = scr("QcT", [4, 66, T], BF16)
        self.KcT = scr("KcT", [4, 66, T], BF16)
        self.Vc = scr("Vc", [T, 256], BF16)
        self.YT = scr("YT", [D, T], BF16)
        NX = self.NT * 4 + 16
        self.NX = NX
        self.XB = scr("XB", [256, T], F32)
        self.GB = scr("GB", [256, T], F32)
        self.ZT = scr("ZT", [256, T], BF16)
        self.EXC = scr("EXC", [128, NX], F32)
        self.EXS = scr("EXS", [128, 256], BF16)
        self.EXH = scr("EXH", [128, 16], F32)
        self.EXCg = scr("EXCg", [256, NX], F32)
        self.EXSg = scr("EXSg", [256, 256], BF16)
        self.EXHg = scr("EXHg", [256, 16], F32)
        self.KcTg = scr("KcTg", [2, 264, T], BF16)
        self.Vcg = scr("Vcg", [2 * T, 256], BF16)
        self.WGU = scr("WGU", [16, 128, 8, 512], BF16)
        self.WO = scr("WO", [128, 8, D], BF16)
        self.MODS = scr("MODS", [L_FULL, 128, 6 * D], F32)
        self.WD = scr("WD", [2, 4, 128, 8, 512], BF16)
        if self.dbg:
            self.dbgmod = scr("dbgmod", [128, 6 * D], F32)
            self.dbgcum = scr("dbgcum", [128, self.NT, 4], F32)

        with ExitStack() as es:
            self.es = es
            self.s = Sched(nc, es)
            self.persistent()
            self.s.flush()
            for l in range(L):
                self.setup_layer(l)
                self.s.barrier(); self.s.flush()
                if self.stop_after == ('setup', l):
                    break
                self.phase_A(l)
                self.s.barrier(); self.s.flush()
                if self.stop_after == ('A', l):
                    break
                self.phase_fox(l)
                self.s.barrier(); self.s.flush()
                if self.stop_after == ('fox', l):
                    break
                self.phase_moe(l, last=(l == L - 1))
                self.s.barrier(); self.s.flush()
            self.s.finish()
        return nc

    def persistent(self):
        nc, es, s = self.nc, self.es, self.s

        def A(name, shape, dtype):
            return es.enter_context(nc.sbuf_tensor(self.un(name), list(shape), dtype))
        self.ident_f = A("ident_f", [128, 128], F32)
        self.ident_b = A("ident_b", [128, 128], BF16)
        self.ones_f = A("ones_f", [128, 128], F32)
        self.ones_b = A("ones_b", [128, 512], BF16)
        self.shm = A("shm", [128, D], F32)
        self.shf = A("shf", [128, D], F32)
        self.A_mix = A("A_mix", [128, D], F32)
        self.A_ffn = A("A_ffn", [128, D], F32)
        self.nfin = A("nfin", [128, D], F32)
        self.csil = A("csil", [128, 8], F32)
        self.exc = A("exc", [128, self.NT * 4 + 16], F32)
        self.cumK = self.exc[:, 0:self.NT * 4].rearrange("p (a b) -> p a b", b=4)
        self.flg = A("flg", [128, 4], F32)
        self.Cbc = A("Cbc", [128, self.NCH, 4], F32)
        self.R_const = s.res("const")
        self.R_mod = s.res("mod")
        self.R_cum = s.res("cum")
        R = self.R_const
        self.memset('pool', self.ones_f[:], 1.0, [R])
        self.memset('pool', self.ones_b[:], 1.0, [R])
        self.asel(self.ident_f[:], self.ones_f[:], [[-1, 128]], ALU.is_equal, 0.0, 0, 1, [R], [R])
        self.cp('pool', self.ident_b[:], self.ident_f[:], [R], [R])
        with ExitStack() as ph:
            ct = ph.enter_context(nc.sbuf_tensor(self.un("ct"), [128, 8], F32))
            Rt = s.res("tmp")
            self.dma('sp', ct[:], self.cT, (), [Rt], owner=Rt)
            self.act(self.csil[:], ct[:], AF.Silu, [Rt], [R])
            self.memset('pool', self.exc[:], 0.0, [self.R_cum])
            self.dma('sp', self.flg[:], self.flags, (), [Rt], owner=Rt)
            Rn = s.res("nfin")
            self.dma('sp', self.nfin[:], self.norm_final.partition_broadcast(128), (), [Rn], owner=Rn)
            for h in range(4):
                for c in range(self.NCH):
                    self.dma('sp', self.KcT[h, 64:66, c * 512:(c + 1) * 512], self.ones_b[0:2, :], [R], (), owner=Rt)
            s.barrier()
            s.flush()

    def mod_ops(self, l, mod, cB, wm, Rwm, pm, Rpm, R, q='sp'):
        pcs = []
        wv = self.w_mod[l].rearrange("(kc p) n -> p kc n", p=128)
        pcs.append(lambda: self.dma(q, mod[:], self.b_mod[l].partition_broadcast(128), (), [R], owner=R))
        for n in range(12):
            sl = n % 2
            pcs.append(lambda n=n, sl=sl: self.dma(q, wm[:, sl], wv[:, :, n * 512:(n + 1) * 512], (), [Rwm[sl]], owner=Rwm[sl]))

            def f(n=n, sl=sl):
                for kc in range(8):
                    self.mm(pm[:], cB[:, kc, :], wm[:, sl, kc, :], kc == 0, kc == 7, [R, Rwm[sl]], [Rpm])
            pcs.append(f)
            pcs.append(lambda n=n: self.tt('dve', mod[:, n * 512:(n + 1) * 512], pm[:], mod[:, n * 512:(n + 1) * 512],
                                           ALU.add, [Rpm, R], [R]))
        return pcs

    def mod_ahead_pieces(self, l, A, px, Rpx):
        s = self.s
        pcs = []
        cB = A("cBa", [128, 8, 128], F32); RcB = s.res("cBa")
        wm = A("wma", [128, 8, 256], F32); Rwm = s.res("wma")
        bm = A("bma", [128, 256], F32); Rbm = s.res("bma")
        wv = self.w_mod[l].rearrange("(kc p) n -> p kc n", p=128)
        pcs.append(lambda: self.cp('dve', cB[:], self.csil[:].unsqueeze(2).to_broadcast([128, 8, 128]), [self.R_const], [RcB]))
        for n in range(24):
            cs = slice(n * 256, (n + 1) * 256)

            def ld(n=n, cs=cs):
                self.dma('pool', wm[:], wv[:, :, cs], (), [Rwm], owner=Rwm)
                self.dma('pool', bm[:], self.b_mod[l, cs].partition_broadcast(128), (), [Rbm], owner=Rbm)
            pcs.append(ld)

            def f(n=n):
                for kc in range(8):
                    self.mm(px[:, 0:256], cB[:, kc, :], wm[:, kc, :], kc == 0, kc == 7, [RcB, Rwm], [Rpx])
                self.tt('dve', bm[:], px[:, 0:256], bm[:], ALU.add, [Rpx, Rbm], [Rbm])
            pcs.append(f)
            pcs.append(lambda n=n, cs=cs: self.dma('pool', self.MODS[l, :, cs], bm[:], [Rbm], (), owner=Rbm))
        return pcs

    def setup_layer(self, l):
        nc, s = self.nc, self.s
        R = self.R_mod
        with ExitStack() as ph:
            def A(name, shape, dtype):
                return ph.enter_context(nc.sbuf_tensor(self.un(name), list(shape), dtype))
            mod = A("mod", [128, 6 * D], F32)
            nb = A("nb", [128, 2, D], F32)
            Rnb = s.res("nb")
            self.dma('sp', nb[:, 0, :], self.norm_mix[l].partition_broadcast(128), (), [Rnb], owner=Rnb)
            self.dma('sp', nb[:, 1, :], self.norm_ffn[l].partition_broadcast(128), (), [Rnb], owner=Rnb)
            if l == 0:
                cB = A("cB", [128, 8, 128], F32)
                wm = A("wm", [128, 2, 8, 512], F32)
                Rwm = [s.res("wm0"), s.res("wm1")]
                pm = ph.enter_context(nc.psum_tensor(self.un("pm"), [128, 512], F32))
                Rpm = s.res("pm0")
                self.cp('dve', cB[:], self.csil[:].unsqueeze(2).to_broadcast([128, 8, 128]), [self.R_const], [R])
                for p in self.mod_ops(l, mod, cB, wm, Rwm, pm, Rpm, R):
                    p()
                self.dma('pool', self.MODS[l], mod[:], [R], (), owner=Rnb)
            else:
                self.dma('sp', mod[:], self.MODS[l], (), [R], owner=R)
            self.stt(self.A_mix[:], mod[:, D:2 * D], 1.0, nb[:, 0, :], ALU.add, ALU.mult, [R, Rnb], [R])
            self.stt(self.A_ffn[:], mod[:, 4 * D:5 * D], 1.0, nb[:, 1, :], ALU.add, ALU.mult, [R, Rnb], [R])
            self.cp('pool', self.shm[:], mod[:, 0:D], [R], [R])
            self.cp('pool', self.shf[:], mod[:, 3 * D:4 * D], [R], [R])
            if self.dbg and l == 0:
                self.dma('sp', self.dbgmod, mod[:], [R], (), owner=Rnb)
            s.barrier()
            s.flush()

    def conv_pieces(self, l, A):
        s = self.s
        pcs = []
        gm = A("gm", [128, D], F32); Rgm = s.res("gm")
        gain = A("gain", [128, 8], F32)
        wst = A("wst", [128, 2, D], F32); Rwst = [s.res("wst0"), s.res("wst1")]
        wob = A("wob", [128, D], BF16); Rwob = s.res("wob0")
        Rcv = s.res("cvdma")

        def p0():
            self.dma('pool', gm[:], self.MODS[l, :, 2 * D:3 * D], (), [Rgm], owner=Rgm)
            self.dma('pool', gain[:], self.gainT[l], (), [Rgm], owner=Rgm)
        pcs.append(p0)
        for fc in range(8):
            sl = fc % 2
            pcs.append(lambda fc=fc, sl=sl: self.dma('pool', wst[:, sl, :], self.w_out[l, fc * 128:(fc + 1) * 128, :], (), [Rwst[sl]], owner=Rwst[sl]))
            pcs.append(lambda fc=fc, sl=sl: self.stt(wob[:], wst[:, sl, :], gain[:, fc:fc + 1], gm[:], ALU.mult, ALU.mult,
                                                     [Rwst[sl], Rgm], [Rwob]))
            pcs.append(lambda fc=fc: self.dma('pool', self.WO[:, fc, :], wob[:], [Rwob], (), owner=Rwob))
        for e in range(16):
            for gi, wsrc in enumerate((self.w_gate, self.w_up)):
                pcs.append(lambda e=e, gi=gi, wsrc=wsrc: self.dma('pool', self.WGU[e][:, :, gi * 256:(gi + 1) * 256],
                                                                  wsrc[l, e].rearrange("(kc p) n -> p kc n", p=128), (), (), owner=Rcv))
            for nh in range(2):
                pcs.append(lambda e=e, nh=nh: self.dma('pool', self.WD[nh, e // 4, :, (e % 4) * 2:(e % 4) * 2 + 2, :],
                                                       self.w_down[l, e, :, nh * 512:(nh + 1) * 512].rearrange("(kc p) n -> p kc n", p=128),
                                                       (), (), owner=Rcv))
        return pcs

    def phase_A(self, l):
        nc, s = self.nc, self.s
        T, NCH = self.T, self.NCH
        xsrc = self.x_in if l == 0 else self.xs
        RC, RM = self.R_const, self.R_mod
        with ExitStack() as ph:
            def A(name, shape, dtype):
                return ph.enter_context(nc.sbuf_tensor(self.un(name), list(shape), dtype))

            def P(name, shape, dtype):
                return ph.enter_context(nc.psum_tensor(self.un(name), list(shape), dtype))
            winb = A("winb", [128, 8, IN_COLS], BF16); Rwin = s.res("winb")
            wstg = A("wstg", [128, 2, IN_COLS], F32); Rwstg = [s.res("wstg0"), s.res("wstg1")]
            for kc in range(8):
                sl = kc % 2
                self.dma('sp', wstg[:, sl, :], self.w_in[l, kc * 128:(kc + 1) * 128, :], (), [Rwstg[sl]], owner=Rwstg[sl])
                self.cp('pool', winb[:, kc, :], wstg[:, sl, :], [Rwstg[sl]], [Rwin])
            negbf = A("negbf", [4, 1], F32)
            Rp = s.res("lrup")
            self.dma('sp', negbf[:], self.fox_bf[l].rearrange("(h o) -> h o", o=1), (), [Rp], owner=Rp)
            self.ts('dve', negbf[:], negbf[:], -1.0, None, ALU.mult, None, [Rp], [Rp])

            xt = A("xt", [128, 4, D], F32); Rxt = [s.res(f"xt{i}") for i in range(4)]
            junk = A("junk", [128, D], BF16); Rjunk = s.res("junk")
            st = A("st", [128, 4, 4], F32); Rst = [s.res(f"st{i}") for i in range(4)]
            u = A("u", [128, 4, D], F32); Ru = [s.res(f"u{i}") for i in range(4)]
            hb = A("hb", [128, 4, D], BF16); Rhb = [s.res(f"hb{i}") for i in range(4)]
            hT = A("hT", [128, 2, 8, 512], BF16); RhT = [s.res("hT0"), s.res("hT1")]
            ptr = [P(f"ptr{i}", [128, 8, 128], BF16) for i in range(2)]; Rptr = [s.res("ptr0"), s.res("ptr1")]
            pp = [P(f"pp{i}", [128, 512], F32) for i in range(4)]; Rpp = [s.res(f"pp{i}") for i in range(4)]
            pq = [P(f"pq{i}", [128, 512], F32) for i in range(2)]; Rpq = [s.res(f"pq{i}") for i in range(2)]
            NST = 4
            stg = A("stg", [128, NST, 512], BF16); Rstg = [s.res(f"stg{i}") for i in range(NST)]
            vstg = A("vstg", [128, 2, 384], BF16); Rvstg = [s.res("vstg0"), s.res("vstg1")]
            fstg = A("fstg", [128, 4, 512], F32); Rfstg = [s.res(f"fstg{i}") for i in range(4)]
            ifs = 0
            fT = A("fT", [4, 512], F32); RfT = s.res("fT")
            spx = A("spx", [4, 512], F32); Rspx = s.res("spx")
            rr_ = A("rr", [4, 512], F32); Rrr = s.res("rr")
            cg = A("cg", [4, 512], F32); Rcg = s.res("cg")
            rhi = A("rhi", [4, 512], BF16); rlo = A("rlo", [4, 512], BF16); Rrh = s.res("rhl")
            rt = A("rt", [4, 512], F32); Rrt = s.res("rt")
            fcar = A("fcar", [4, 1], F32); Rfc = s.res("fcar")
            cbt = A("cbt", [4, 128], F32); Rcbt = s.res("cbt")
            self.memset('pool', fcar[:], 0.0, [Rfc])
            self.memset('pool', rt[:], 1.0, [Rrt])
            ist = 0
            ipp = 0
            conv = self.conv_pieces(l, A)
            if getattr(self, 'verbose', False):
                print("phase A sbuf remaining", nc.sbuf_bytes_remaining)
            nslots = NCH * 17
            cvi = 0
            cvslot = 0

            def conv_tick():
                nonlocal cvi, cvslot
                cvslot += 1
                tgt = (len(conv) * cvslot) // nslots
                while cvi < min(tgt, len(conv)):
                    conv[cvi]()
                    cvi += 1
            FM = [('qa', 0), ('qa', 128), ('qa', 256), ('qa', 384), ('ka', 512),
                  ('xb', 768), ('xb', 896), ('gb', 1024), ('gb', 1152),
                  ('qc', 1280), ('qc', 1408), ('kc', 1536), ('kc', 1664)]
            for c in range(NCH):
                hs_ = c % 2
                cols = slice(c * 512, (c + 1) * 512)
                for i in range(4):
                    tok = c * 512 + i * 128
                    sl = i
                    pslt = (c * 4 + i) % 2
                    self.dma('sp', xt[:, sl, :], xsrc[tok:tok + 128, :], (), [Rxt[sl]], owner=Rxt[sl])
                    self.act(junk[:], xt[:, sl, :], AF.Square, [Rxt[sl]], [Rjunk, Rst[sl]], accum_out=st[:, sl, 0:1])
                    self.ts('dve', st[:, sl, 1:2], st[:, sl, 0:1], 1.0 / D, EPS, ALU.mult, ALU.add, [Rst[sl]], [Rst[sl]])
                    self.act(st[:, sl, 2:3], st[:, sl, 1:2], AF.Sqrt, [Rst[sl]], [Rst[sl]])
                    self.recip(st[:, sl, 3:4], st[:, sl, 2:3], [Rst[sl]], [Rst[sl]])
                    self.stt(u[:, sl, :], xt[:, sl, :], st[:, sl, 3:4], self.A_mix[:], ALU.mult, ALU.mult,
                             [Rxt[sl], Rst[sl], RM], [Ru[sl]])
                    self.tt('pool', hb[:, sl, :], u[:, sl, :], self.shm[:], ALU.add, [Ru[sl], RM], [Rhb[sl]])
                    for dc in range(8):
                        self.tr(ptr[pslt][:, dc, :], hb[:, sl, dc * 128:(dc + 1) * 128], self.ident_b[:], [Rhb[sl], RC], [Rptr[pslt]])
                    self.cp('act', hT[:, hs_, :, i * 128:(i + 1) * 128], ptr[pslt][:], [Rptr[pslt]], [RhT[hs_]])
                for i in range(4):
                    tok = c * 512 + i * 128
                    b = ipp % 4; ipp += 1
                    vs = i % 2
                    for dc in range(8):
                        self.mm(pp[b][:, 0:128], hT[:, hs_, dc, i * 128:(i + 1) * 128], winb[:, dc, 640:768], dc == 0, dc == 7,
                                [RhT[hs_], Rwin], [Rpp[b]])
                    for dc in range(8):
                        self.mm(pp[b][:, 128:384], hT[:, hs_, dc, i * 128:(i + 1) * 128], winb[:, dc, 1792:2048], dc == 0, dc == 7,
                                [RhT[hs_], Rwin], [Rpp[b]])
                    self.evac(vstg[:, vs, :], pp[b][:, 0:384], [Rpp[b]], [Rvstg[vs]])
                    self.dma('pool', self.Va[tok:tok + 128, :], vstg[:, vs, 0:128], [Rvstg[vs]], (), owner=Rvstg[vs])
                    self.dma('pool', self.Vc[tok:tok + 128, :], vstg[:, vs, 128:384], [Rvstg[vs]], (), owner=Rvstg[vs])
                    conv_tick()
                for (nm, col) in FM:
                    b = ipp % 4; ipp += 1
                    for dc in range(8):
                        self.mm(pp[b][:], winb[:, dc, col:col + 128], hT[:, hs_, dc, :], dc == 0, dc == 7,
                                [Rwin, RhT[hs_]], [Rpp[b]])
                    if nm in ('qa', 'ka', 'qc', 'kc'):
                        si = ist % NST; ist += 1
                        self.evac(stg[:, si, :], pp[b][:], [Rpp[b]], [Rstg[si]], scale=(0.125 if nm[0] == 'q' else None))
                        if nm == 'qa':
                            self.dma('pool', self.QaT[col:col + 128, cols], stg[:, si, :], [Rstg[si]], (), owner=Rstg[si])
                        elif nm == 'ka':
                            self.dma('pool', self.KaT[:, cols], stg[:, si, :], [Rstg[si]], (), owner=Rstg[si])
                        else:
                            dst = self.QcT if nm == 'qc' else self.KcT
                            h0 = (col - (1280 if nm == 'qc' else 1536)) // 64
                            self.dma('pool', dst[h0, 0:64, cols], stg[0:64, si, :], [Rstg[si]], (), owner=Rstg[si])
                            self.dma('pool', dst[h0 + 1, 0:64, cols], stg[64:128, si, :], [Rstg[si]], (), owner=Rstg[si])
                    else:
                        k = (col - (768 if nm == 'xb' else 1024)) // 128
                        fi = ifs % 4; ifs += 1
                        self.evac(fstg[:, fi, :], pp[b][:], [Rpp[b]], [Rfstg[fi]])
                        dst = self.XB if nm == 'xb' else self.GB
                        self.dma('pool', dst[k * 128:(k + 1) * 128, cols], fstg[:, fi, :], [Rfstg[fi]], (), owner=Rfstg[fi])
                        if nm == 'xb' and c == NCH - 1:
                            o_ = self.NT * 4 + 4 + k * 3
                            self.cp('dve', self.exc[:, o_:o_ + 3], fstg[:, fi, 509:512], [Rfstg[fi]], [self.R_cum])
                    conv_tick()
                b = ipp % 4; ipp += 1
                for dc in range(8):
                    self.mm(pp[b][0:4, :], winb[:, dc, 2048:2052], hT[:, hs_, dc, :], dc == 0, dc == 7, [Rwin, RhT[hs_]], [Rpp[b]])
                self.evac(fT[:], pp[b][0:4, :], [Rpp[b]], [RfT], eng='dve')
                self.act(spx[:], fT[:], AF.Exp, [RfT, Rp], [Rspx], bias=negbf[:], scale=-1.0)
                self.act(spx[:], spx[:], AF.Ln, [Rspx], [Rspx], bias=1.0)
                self.scan(rr_[:], rt[:], spx[:], 0.0, ALU.mult, ALU.subtract, [Rspx, Rrt], [Rrr])
                self.cp('dve', cbt[:], fcar[:].to_broadcast([4, 128]), [Rfc], [Rcbt])
                self.mm(pq[0][:, 0:4], cbt[:], self.ident_f[0:4, 0:4], True, True, [Rcbt, RC], [Rpq[0]])
                self.cp('dve', self.Cbc[:, c, :], pq[0][:, 0:4], [Rpq[0]], [self.R_cum])
                self.ts('dve', cg[:], rr_[:], fcar[:], None, ALU.add, None, [Rrr, Rfc], [Rcg])
                self.cp('dve', fcar[:], cg[:, 511:512], [Rcg], [Rfc])
                for i in range(4):
                    self.tr(pq[1][:, i * 4:(i + 1) * 4], cg[:, i * 128:(i + 1) * 128], self.ident_f[0:4, 0:4], [Rcg, RC], [Rpq[1]])
                self.cp('dve', self.cumK[:, c * 4:(c + 1) * 4, :], pq[1][:, 0:16].rearrange("p (a b) -> p a b", a=4),
                        [Rpq[1]], [self.R_cum])
                self.cp('dve', rhi[:], rr_[:], [Rrr], [Rrh])
                self.tt('dve', rlo[:], rr_[:], rhi[:], ALU.subtract, [Rrr, Rrh], [Rrh])
                self.dma('pool', self.QcT[:, 64, cols], rhi[:], [Rrh], (), owner=Rrh)
                self.dma('pool', self.QcT[:, 65, cols], rlo[:], [Rrh], (), owner=Rrh)
            while cvi < len(conv):
                conv[cvi]()
                cvi += 1
            self.cp('dve', cbt[:], fcar[:].to_broadcast([4, 128]), [Rfc], [Rcbt])
            self.mm(pq[0][:, 0:4], cbt[:], self.ident_f[0:4, 0:4], True, True, [Rcbt, RC], [Rpq[0]])
            self.cp('dve', self.exc[:, self.NT * 4:self.NT * 4 + 4], pq[0][:, 0:4], [Rpq[0]], [self.R_cum])
            Rex = s.res("exc")
            self.dma('sp', self.EXC, self.exc[:], [self.R_cum], (), owner=Rex)
            if self.dbg:
                self.dma('sp', self.dbgcum, self.cumK, [self.R_cum], (), owner=Rrt)
            s.barrier()
            self.dma('sp', self.EXS[:, 0:128], self.KaT[:, T - 128:T], (), (), owner=Rex)
            self.dma('sp', self.EXS[:, 128:256], self.Va[T - 128:T, :], (), (), owner=Rex)
            for hp in range(2):
                self.allgather(self.KcT[2 * hp:2 * hp + 2].rearrange("h r t -> (h r) t"), self.KcTg[hp], "cc_k")
            self.allgather(self.Vc, self.Vcg, "cc_v")
            self.allgather(self.EXC, self.EXCg, "cc_c")
            self.allgather(self.EXS, self.EXSg, "cc_s")
            s.barrier()
            s.flush()

    def lru_pieces(self, l, A, px, Rpx):
        s = self.s
        T, NCH, NT = self.T, self.NCH, self.NT
        RC = self.R_const
        pcs = []
        cw = A("cw", [128, 2, 4], F32); cb = A("cb", [128, 2], F32)
        ba = A("ba", [128, 2], F32); bx = A("bx", [128, 2], F32); lam = A("lam", [128, 2], F32)
        cL = A("cL", [128, 2], F32); cL2 = A("cL2", [128, 2], F32); tmpl = A("tmpl", [128, 2], F32)
        wabd = A("wabd", [128, 2, 128], F32); wxbd = A("wxbd", [128, 2, 128], F32)
        Rp = s.res("lrup")
        xbT = A("xbT", [128, 2, 2, 515], F32); Rxb = [s.res("xb0"), s.res("xb1")]
        gbT = A("gbT", [128, 2, 2, 512], F32); Rgb = [s.res("gb0"), s.res("gb1")]
        xc = A("xc", [128, 512], F32); Rxc = s.res("xc")
        rg = A("rg", [128, 512], F32); Rrg = s.res("rg")
        ig = A("ig", [128, 512], F32); Rig = s.res("ig")
        aa = A("aa", [128, 512], F32); Raa = s.res("aa")
        sq = A("sq", [128, 512], F32); Rsq = s.res("sq")
        uu = A("uu", [128, 512], F32); Ruu = s.res("uu")
        hs = A("hs", [128, 512], F32); Rhs = s.res("hs")
        ac = A("ac", [128, 512], F32); Rac = s.res("ac")
        zz = A("zz", [128, 512], F32); Rzz = s.res("zz")
        gg = A("gg", [128, 512], F32); Rgg = s.res("gg")
        g2 = A("g2", [128, 512], F32); Rg2 = s.res("g2")
        hcar = A("hcar", [128, 2], F32); Rhc = s.res("hcar")
        acar = A("acar", [128, 2], F32); Rac2 = s.res("acar")
        exh = A("exh", [128, 16], F32); Rexh = s.res("exh")
        self.lru_exh = (exh, Rexh)
        lstg = A("lstg", [128, 2, 512], BF16); Rstg = [s.res("lstg0"), s.res("lstg1")]
        ctail = A("ctail", [128, 2, 3], F32); Rct = s.res("ctail")

        def init():
            self.memset('pool', wabd[:], 0.0, [Rp])
            self.memset('pool', wxbd[:], 0.0, [Rp])
            for (dst, src) in ((cw, self.convT), (cb, self.convb), (ba, self.lru_ba), (bx, self.lru_bx), (lam, self.lru_lam)):
                self.dma('pool', dst[:], src[l], (), [Rp], owner=Rp)
            for b in range(8):
                k, q = b // 4, (b % 4) * 32
                self.dma('pool', wabd[q:q + 32, k, q:q + 32], self.lru_wa[l, b], (), [Rp], owner=Rp)
                self.dma('pool', wxbd[q:q + 32, k, q:q + 32], self.lru_wx[l, b], (), [Rp], owner=Rp)
            self.memset('pool', zz[:], 0.0, [Rzz])
            self.memset('pool', hcar[:], 0.0, [Rhc])
            self.memset('pool', acar[:], 1.0, [Rac2])
            self.memset('pool', exh[:], 0.0, [Rexh])
            o_ = self.NT * 4 + 4
            self.dma('pool', ctail[:], self.EXCg[0:128, o_:o_ + 6].rearrange("p (a b) -> p a b", a=2), (), [Rct], owner=Rct)
        pcs.append(init)

        def init2():
            self.act(tmpl[:], lam[:], AF.Exp, [Rp], [Rp], scale=-1.0)
            self.act(tmpl[:], tmpl[:], AF.Ln, [Rp], [Rp], bias=1.0)
            self.ts('dve', xbT[:, 0, :, 0:3], ctail[:], self.flg[:, 0:1], None, ALU.mult, None, [Rct, RC], [Rxb[0]])
        pcs.append(init2)

        def init3():
            self.ts('dve', cL[:], tmpl[:], -8.0, None, ALU.mult, None, [Rp], [Rp])
            self.ts('dve', cL2[:], tmpl[:], -16.0, None, ALU.mult, None, [Rp], [Rp])
        pcs.append(init3)
        for c in range(NCH):
            sl = c % 2
            cols = slice(c * 512, (c + 1) * 512)

            def ld(c=c):
                sl_ = c % 2
                cols_ = slice(c * 512, (c + 1) * 512)
                if c > 0:
                    self.cp('pool', xbT[:, sl_, :, 0:3], xbT[:, 1 - sl_, :, 512:515], [Rxb[1 - sl_]], [Rxb[sl_]])
                self.dma('sp', xbT[:, sl_, :, 3:515], self.XB[:, cols_].rearrange("(k p) t -> p k t", p=128), (), [Rxb[sl_]], owner=Rxb[sl_])
                self.dma('sp', gbT[:, sl_], self.GB[:, cols_].rearrange("(k p) t -> p k t", p=128), (), [Rgb[sl_]], owner=Rgb[sl_])
            if c == 0:
                pcs.append(ld)
            if c + 1 < NCH:
                pcs.append(lambda c=c: ld(c + 1))
            for k in range(2):
                def s1(sl=sl, k=k):
                    self.ts('dve', xc[:], xbT[:, sl, k, 0:512], cw[:, k, 0:1], cb[:, k:k + 1], ALU.mult, ALU.add, [Rxb[sl], Rp], [Rxc])
                    for j in range(1, 4):
                        self.stt(xc[:], xbT[:, sl, k, j:j + 512], cw[:, k, j:j + 1], xc[:], ALU.mult, ALU.add, [Rxb[sl], Rp, Rxc], [Rxc])
                    self.act(g2[:], gbT[:, sl, k, :], AF.Square, [Rgb[sl]], [Rg2])

                def s2(sl=sl, k=k):
                    self.mm(px[:], wabd[:, k, :], xc[:], True, True, [Rp, Rxc], [Rpx])
                    self.ts('dve', g2[:], g2[:], 0.044715, 1.0, ALU.mult, ALU.add, [Rg2], [Rg2])

                def s3(sl=sl, k=k):
                    self.act(rg[:], px[:], AF.Sigmoid, [Rpx, Rp], [Rrg], bias=ba[:, k:k + 1])
                    self.tt('pool', g2[:], g2[:], gbT[:, sl, k, :], ALU.mult, [Rg2, Rgb[sl]], [Rg2])

                def s4(sl=sl, k=k):
                    self.mm(px[:], wxbd[:, k, :], xc[:], True, True, [Rp, Rxc], [Rpx])
                    self.act(aa[:], rg[:], AF.Exp, [Rrg, Rp], [Raa], scale=cL[:, k:k + 1])
                    self.act(sq[:], rg[:], AF.Exp, [Rrg, Rp], [Rsq], scale=cL2[:, k:k + 1])

                def s5(sl=sl, k=k):
                    self.act(ig[:], px[:], AF.Sigmoid, [Rpx, Rp], [Rig], bias=bx[:, k:k + 1])
                    self.act(sq[:], sq[:], AF.Sqrt, [Rsq], [Rsq], bias=1.0, scale=-1.0)
                    self.act(gg[:], g2[:], AF.Sigmoid, [Rg2], [Rgg], scale=1.5957691216057308)
                    self.scan(ac[:], aa[:], zz[:], acar[:, k:k + 1], ALU.mult, ALU.add, [Raa, Rzz, Rac2], [Rac])
                    self.cp('dve', acar[:, k:k + 1], ac[:, 511:512], [Rac], [Rac2])

                def s6(sl=sl, k=k):
                    self.tt('pool', uu[:], ig[:], xc[:], ALU.mult, [Rig, Rxc], [Ruu])
                    self.tt('pool', uu[:], uu[:], sq[:], ALU.mult, [Ruu, Rsq], [Ruu])
                    self.tt('pool', gg[:], gg[:], gbT[:, sl, k, :], ALU.mult, [Rgg, Rgb[sl]], [Rgg])

                def s7(sl=sl, k=k):
                    self.scan(hs[:], aa[:], uu[:], hcar[:, k:k + 1], ALU.mult, ALU.add, [Raa, Ruu, Rhc], [Rhs])
                    self.cp('dve', hcar[:, k:k + 1], hs[:, 511:512], [Rhs], [Rhc])
                    self.tt('pool', lstg[:, 1, :], ac[:], gg[:], ALU.mult, [Rac, Rgg], [Rstg[1]])

                def s8(sl=sl, k=k, cols=cols):
                    self.tt('dve', lstg[:, 0, :], hs[:], gg[:], ALU.mult, [Rhs, Rgg], [Rstg[0]])
                    self.dma('pool', self.ZT[k * 128:(k + 1) * 128, cols], lstg[:, 1, :], [Rstg[1]], (), owner=Rstg[1])

                def s9(sl=sl, k=k, cols=cols):
                    self.dma('pool', self.YT[512 + k * 128:512 + (k + 1) * 128, cols], lstg[:, 0, :], [Rstg[0]], (), owner=Rstg[0])
                pcs.extend([s1, s2, s3, s4, s5, s6, s7, s8, s9])

        def fin():
            self.cp('dve', exh[:, 0:2], hcar[:], [Rhc], [Rexh])
        pcs.append(fin)
        return pcs

    def _normalize(self, po, Rpo, pb, Rpb, rd, Rrd, osb, Rosb, out_ap, Rout, sink_ap=None):
        if sink_ap is not None:
            self.tt('dve', rd[64:65, :], po[64:65, :], sink_ap, ALU.add, [Rpo, self.R_const], [Rrd])
            self.recip(rd[64:65, :], rd[64:65, :], [Rrd], [Rrd])
        else:
            self.recip(rd[64:65, :], po[64:65, :], [Rpo], [Rrd])
        self.mm(pb[0:64, :], self.ones_f[64:65, 0:64], rd[64:65, :], True, True, [self.R_const, Rrd], [Rpb])
        self.cp('act', osb[0:64, :], po[0:64, :], [Rpo], [Rosb])
        i0, i1 = osb[0:64, :], pb[0:64, :]
        if len(out_ap.shape) == 3:
            i0 = i0.rearrange("p (a b) -> p a b", a=4)
            i1 = i1.rearrange("p (a b) -> p a b", a=4)
        self.tt('dve', out_ap, i0, i1, ALU.mult, [Rosb, Rpb], [Rout])

    def phase_swa(self, l):
        nc, s = self.nc, self.s
        T, NCH, NT = self.T, self.NCH, self.NT
        RC = self.R_const
        with ExitStack() as ph:
            def A(name, shape, dtype):
                return ph.enter_context(nc.sbuf_tensor(self.un(name), list(shape), dtype))

            def P(name, shape, dtype):
                return ph.enter_context(nc.psum_tensor(self.un(name), list(shape), dtype))
            amask = A("amask", [128, 2, 2, 4, 128], BF16); Rm = s.res("amask")
            dd = A("dd", [128, 128], F32); d2 = A("d2", [128, 128], F32); Rt = s.res("tmp")
            s.op('pool', lambda e: e.iota(dd[:], [[1, 128]], base=0, channel_multiplier=-1,
                                          allow_small_or_imprecise_dtypes=True), (), [Rt])
            for hq in range(8):
                g, hh = hq // 4, hq % 4
                slope = 2.0 ** (-(hq + 1))
                self.ts('pool', d2[:], dd[:], -slope, None, ALU.mult, None, [Rt], [Rt])
                self.asel(amask[:, g, 1, hh, :], d2[:], [[1, 128]], ALU.is_ge, NEG, 0, -1, [Rt], [Rm])
                self.ts('pool', d2[:], dd[:], 128.0, -slope, ALU.add, ALU.mult, [Rt], [Rt])
                self.asel(amask[:, g, 0, hh, :], d2[:], [[-1, 128]], ALU.is_ge, NEG, -1, 1, [Rt], [Rm])
            sk = A("sk", [64, 8], F32); sinkb = A("sinkb", [64, 2, 4, 128], F32); Rsk = s.res("sk")
            self.dma('sp', sk[:], self.sinks[l].partition_broadcast(64), (), [Rsk], owner=Rsk)
            self.act(sk[:], sk[:], AF.Exp, [Rsk], [Rsk])
            for g in range(2):
                self.cp('dve', sinkb[:, g], sk[:, g * 4:(g + 1) * 4].unsqueeze(2).to_broadcast([64, 4, 128]), [Rsk], [RC])
            kat = A("kat", [64, 2, 128 + T], BF16); Rkat = [s.res("kat0"), s.res("kat1")]
            vaa = A("vaa", [128, 2, NT + 1, 65], BF16); Rvaa = s.res("vaa")
            qat = A("qat", [64, 2, 4, 512], BF16); Rqat = [s.res("qat0"), s.res("qat1")]
            ya = A("ya", [64, 2, 4, 512], BF16); Rya = [s.res("ya0"), s.res("ya1")]
            pt = A("pt", [128, 4, 512], BF16); Rpt = [s.res(f"pt{i}") for i in range(4)]
            rd = A("rd", [128, 2, 512], F32); Rrd = [s.res("rd0"), s.res("rd1")]
            osb = A("osb", [64, 2, 512], F32); Rosb = [s.res("osb0"), s.res("osb1")]
            ps = [P(f"ps{i}", [128, 512], F32) for i in range(4)]; Rps = [s.res(f"ps{i}") for i in range(4)]
            po = [P(f"po{i}", [128, 512], F32) for i in range(2)]; Rpo = [s.res(f"po{i}") for i in range(2)]
            pb = [P(f"pb{i}", [128, 512], F32) for i in range(2)]; Rpb = [s.res(f"pb{i}") for i in range(2)]
            self.memset('pool', vaa[:], 1.0, [Rvaa])
            units = [(g, c, i) for g in range(2) for c in range(NCH) for i in range(4)]

            def stage1(u):
                g, c, i = units[u]
                sl = c % 2
                if c == 0 and i == 0:
                    gs = g % 2
                    self.dma('sp', kat[:, gs, 128:], self.KaT[g * 64:(g + 1) * 64, :], (), [Rkat[gs]], owner=Rkat[gs])
                    self.dma('sp', kat[:, gs, 0:128], self.EXSg[g * 64:(g + 1) * 64, 0:128], (), [Rkat[gs]], owner=Rkat[gs])
                    self.dma('sp', vaa[:, gs, 1:, 0:64], self.Va[:, g * 64:(g + 1) * 64].rearrange("(j p) d -> p j d", p=128),
                             (), [Rvaa], owner=Rvaa)
                    self.dma('sp', vaa[:, gs, 0, 0:64], self.EXSg[0:128, 128 + g * 64:128 + (g + 1) * 64], (), [Rvaa], owner=Rvaa)
                if i == 0:
                    cols = slice(c * 512, (c + 1) * 512)
                    self.dma('sp', qat[:, sl], self.QaT[g * 256:(g + 1) * 256, cols].rearrange("(h d) t -> d h t", d=64),
                             (), [Rqat[sl]], owner=Rqat[sl])
                n = c * 4 + i
                for kb in range(2):
                    blk = n + kb
                    b = (u % 2) * 2 + kb
                    self.mm(ps[b][:].rearrange("p (a b) -> p a b", a=4), kat[:, g % 2, blk * 128:(blk + 1) * 128],
                            qat[:, sl, :, i * 128:(i + 1) * 128], True, False, [Rkat[g % 2], Rqat[sl]], [Rps[b]])
                    self.mm(ps[b][:].rearrange("p (a b) -> p a b", a=4), self.ident_b[:], amask[:, g, kb], False, True,
                            [RC, Rm], [Rps[b]])

            def stage2(u):
                g, c, i = units[u]
                n = c * 4 + i
                o = u % 2
                for kb in range(2):
                    blk = n + kb
                    b = (u % 2) * 2 + kb
                    if blk == 0:
                        self.act(pt[:, b, :], ps[b][:], AF.Exp, [Rps[b], RC], [Rpt[b]], bias=self.flg[:, 1:2])
                    else:
                        self.act(pt[:, b, :], ps[b][:], AF.Exp, [Rps[b]], [Rpt[b]])
                for kb in range(2):
                    blk = n + kb
                    b = (u % 2) * 2 + kb
                    self.mm(po[o][0:65, :], vaa[:, g % 2, blk, 0:65], pt[:, b, :], kb == 0, kb == 1, [Rvaa, Rpt[b]], [Rpo[o]])

            def stage3(u):
                g, c, i = units[u]
                sl = c % 2
                o = u % 2
                rdo = rd[0:64, o, :]
                self.tt('dve', rdo, pb[o][0:64, :], sinkb[:, g].rearrange("p a b -> p (a b)"), ALU.add, [Rpb[o], RC], [Rrd[o]])
                self.recip(rdo, rdo, [Rrd[o]], [Rrd[o]])
                self.cp('act', osb[0:64, o, :], po[o][0:64, :], [Rpo[o]], [Rosb[o]])
                self.tt('dve', ya[:, sl, :, i * 128:(i + 1) * 128], osb[0:64, o, :].rearrange("p (a b) -> p a b", a=4),
                        rdo.rearrange("p (a b) -> p a b", a=4), ALU.mult, [Rosb[o], Rrd[o]], [Rya[sl]])
                if i == 3:
                    cols = slice(c * 512, (c + 1) * 512)
                    self.dma('pool', self.YT[g * 256:(g + 1) * 256, cols].rearrange("(h d) t -> d h t", d=64), ya[:, sl],
                             [Rya[sl]], (), owner=Rya[sl])
            NU = len(units)
            for q in range(NU + 2):
                if q < NU:
                    stage1(q)
                if 0 <= q - 1 < NU:
                    stage2(q - 1)
                if 0 <= q - 2 < NU:
                    stage3(q - 2)
            s.barrier()
            s.flush()

    def phase_fox(self, l):
        nc, s = self.nc, self.s
        T, NCH, NT = self.T, self.NCH, self.NT
        RC = self.R_const
        with ExitStack() as ph:
            def A(name, shape, dtype):
                return ph.enter_context(nc.sbuf_tensor(self.un(name), list(shape), dtype))

            def P(name, shape, dtype):
                return ph.enter_context(nc.psum_tensor(self.un(name), list(shape), dtype))
            fmask = A("fmask", [128, 4, 512], BF16); Rm = s.res("fmask")
            zt = A("zt", [128, 512], F32); Rt = s.res("tmp")
            self.memset('pool', zt[:], 0.0, [Rt])
            for m in range(4):
                self.asel(fmask[:, m, :], zt[:], [[1, 512]], ALU.is_ge, NEG, -128 * m, -1, [Rt], [Rm])
            kt = A("kt", [66, 2, 2 * T], BF16); Rkt = [s.res("kt0"), s.res("kt1")]
            vca = A("vca", [128, 2, 2 * NT, 65], BF16); Rvca = [s.res("vca0"), s.res("vca1")]
            cumP = A("cumP", [128, NT * 4 + 16], F32); Rcp = s.res("cumP")
            self.dma('sp', cumP[:], self.EXCg[0:128, :], (), [Rcp], owner=Rcp)
            cpv = cumP[:, 0:NT * 4].rearrange("p (a b) -> p a b", b=4)
            poff = A("poff", [128, 4], F32)
            self.ts('dve', poff[:], cumP[:, NT * 4:NT * 4 + 4], self.flg[:, 1:2], None, ALU.add, None, [Rcp, RC], [Rcp])
            cq = A("cq", [128, 2, 1], F32); Rcq = [s.res("cq0"), s.res("cq1")]
            qa = A("qa", [66, 2, 512], BF16); Rqa = [s.res("qa0"), s.res("qa1")]
            bt = A("bt", [128, 2, 2 * NT], F32); Rbt = [s.res("bt0"), s.res("bt1")]
            yc = A("yc", [64, 2, 512], BF16); Ryc = [s.res("yc0"), s.res("yc1")]
            pt = A("pt", [128, 3, 512], BF16); Rpt = [s.res(f"pt{i}") for i in range(3)]
            rd = A("rd", [128, 2, 512], F32); Rrd = [s.res("rd0"), s.res("rd1")]
            osb = A("osb", [64, 2, 512], F32); Rosb = [s.res("osb0"), s.res("osb1")]
            ps = [P(f"ps{i}", [128, 512], F32) for i in range(3)]; Rps = [s.res(f"ps{i}") for i in range(3)]
            po = [P(f"po{i}", [128, 512], F32) for i in range(2)]; Rpo = [s.res(f"po{i}") for i in range(2)]
            pb = [P(f"pb{i}", [128, 512], F32) for i in range(2)]; Rpb = [s.res(f"pb{i}") for i in range(2)]
            px = P("px", [128, 512], F32); Rpx = s.res("px")
            self.memset('pool', vca[:], 1.0, [Rvca[0], Rvca[1]])
            amask = A("amask", [128, 2, 2, 4, 128], BF16); Rma = s.res("amask")
            dd = A("dd", [128, 128], F32); d2 = A("d2", [128, 128], F32); Rtt = s.res("tmp2")
            s.op('pool', lambda e: e.iota(dd[:], [[1, 128]], base=0, channel_multiplier=-1,
                                          allow_small_or_imprecise_dtypes=True), (), [Rtt])
            for hq in range(8):
                g, hh = hq // 4, hq % 4
                slope = 2.0 ** (-(hq + 1))
                self.ts('pool', d2[:], dd[:], -slope, None, ALU.mult, None, [Rtt], [Rtt])
                self.asel(amask[:, g, 1, hh, :], d2[:], [[1, 128]], ALU.is_ge, NEG, 0, -1, [Rtt], [Rma])
                self.ts('pool', d2[:], dd[:], 128.0, -slope, ALU.add, ALU.mult, [Rtt], [Rtt])
                self.asel(amask[:, g, 0, hh, :], d2[:], [[-1, 128]], ALU.is_ge, NEG, -1, 1, [Rtt], [Rma])
            sk = A("sk", [64, 8], F32); sinkb = A("sinkb", [64, 2, 4, 128], F32); Rsk = s.res("sk")
            self.dma('sp', sk[:], self.sinks[l].partition_broadcast(64), (), [Rsk], owner=Rsk)
            self.act(sk[:], sk[:], AF.Exp, [Rsk], [Rsk])
            for g in range(2):
                self.cp('dve', sinkb[:, g], sk[:, g * 4:(g + 1) * 4].unsqueeze(2).to_broadcast([64, 4, 128]), [Rsk], [RC])
            kat = A("kat", [64, 2, 128 + T], BF16); Rkat = [s.res("kat0"), s.res("kat1")]
            vaa = A("vaa", [128, 2, NT + 1, 65], BF16); Rvaa = [s.res("vaa0"), s.res("vaa1")]
            qat = A("qat", [64, 2, 4, 512], BF16); Rqat = [s.res("qat0"), s.res("qat1")]
            ya = A("ya", [64, 2, 4, 512], BF16); Rya = [s.res("ya0"), s.res("ya1")]
            self.memset('pool', vaa[:], 1.0, [Rvaa[0], Rvaa[1]])
            main_side = self.lru_pieces(l, A, px, Rpx)
            if l + 1 < self.L:
                main_side = main_side + self.mod_ahead_pieces(l + 1, A, px, Rpx)
            conv_side = []
            side = []
            i1 = i2 = 0
            while i1 < len(main_side) or i2 < len(conv_side):
                if i1 < len(main_side):
                    side.append(main_side[i1]); i1 += 1
                if i2 < len(conv_side):
                    side.append(conv_side[i2]); i2 += 1
            jobs = []
            for g in range(2):
                for c in range(NCH):
                    for i in range(4):
                        for kb in range(2):
                            jobs.append((-1 - g, c, i * 2 + kb, 2))
            for h in range(4):
                for c in range(NCH):
                    nj = NT + 4 * c + 4
                    for j in range(nj):
                        jobs.append((h, c, j, nj))
            LA = 2
            chunk_idx = {}
            for q, (h, c, j, nj) in enumerate(jobs):
                key = (h, c, j // 2) if h < 0 else (h, c)
                if key not in chunk_idx:
                    chunk_idx[key] = len(chunk_idx)

            def swa1(q):
                h, c, j, _ = jobs[q]
                g = -1 - h
                i, kb = j // 2, j % 2
                sl = (g * NCH + c) % 2
                if c == 0 and j == 0:
                    self.dma('sp', kat[:, g, 128:], self.KaT[g * 64:(g + 1) * 64, :], (), [Rkat[g]], owner=Rkat[g])
                    self.dma('sp', kat[:, g, 0:128], self.EXSg[g * 64:(g + 1) * 64, 0:128], (), [Rkat[g]], owner=Rkat[g])
                    self.dma('sp', vaa[:, g, 1:, 0:64], self.Va[:, g * 64:(g + 1) * 64].rearrange("(j p) d -> p j d", p=128),
                             (), [Rvaa[g]], owner=Rvaa[g])
                    self.dma('sp', vaa[:, g, 0, 0:64], self.EXSg[0:128, 128 + g * 64:128 + (g + 1) * 64], (), [Rvaa[g]], owner=Rvaa[g])
                if j == 0:
                    cols = slice(c * 512, (c + 1) * 512)
                    self.dma('sp', qat[:, sl], self.QaT[g * 256:(g + 1) * 256, cols].rearrange("(h d) t -> d h t", d=64),
                             (), [Rqat[sl]], owner=Rqat[sl])
                blk = c * 4 + i + kb
                b = q % 3
                self.mm(ps[b][:].rearrange("p (a b) -> p a b", a=4), kat[:, g, blk * 128:(blk + 1) * 128],
                        qat[:, sl, :, i * 128:(i + 1) * 128], True, False, [Rkat[g], Rqat[sl]], [Rps[b]])
                self.mm(ps[b][:].rearrange("p (a b) -> p a b", a=4), self.ident_b[:], amask[:, g, kb], False, True,
                        [RC, Rma], [Rps[b]])

            def swa2(q):
                h, c, j, _ = jobs[q]
                g = -1 - h
                i, kb = j // 2, j % 2
                blk = c * 4 + i + kb
                b = q % 3
                o = chunk_idx[(h, c, i)] % 2
                if blk == 0:
                    self.act(pt[:, b, :], ps[b][:], AF.Exp, [Rps[b], RC], [Rpt[b]], bias=self.flg[:, 1:2])
                else:
                    self.act(pt[:, b, :], ps[b][:], AF.Exp, [Rps[b]], [Rpt[b]])
                self.mm(po[o][0:64, :], vaa[:, g, blk, 0:64], pt[:, b, :], kb == 0, kb == 1, [Rvaa[g], Rpt[b]], [Rpo[o]])
                self.mm(pb[o][0:64, :], self.ones_b[:, 0:64], pt[:, b, :], kb == 0, kb == 1, [RC, Rpt[b]], [Rpb[o]])

            def swa3(q):
                h, c, j, _ = jobs[q]
                if j % 2 != 1:
                    return
                g = -1 - h
                i = j // 2
                sl = (g * NCH + c) % 2
                o = chunk_idx[(h, c, i)] % 2
                rdo = rd[0:64, o, :]
                self.tt('dve', rdo, pb[o][0:64, :], sinkb[:, g].rearrange("p a b -> p (a b)"), ALU.add, [Rpb[o], RC], [Rrd[o]])
                self.recip(rdo, rdo, [Rrd[o]], [Rrd[o]])
                self.cp('act', osb[0:64, o, :], po[o][0:64, :], [Rpo[o]], [Rosb[o]])
                self.tt('dve', ya[:, sl, :, i * 128:(i + 1) * 128], osb[0:64, o, :].rearrange("p (a b) -> p a b", a=4),
                        rdo.rearrange("p (a b) -> p a b", a=4), ALU.mult, [Rosb[o], Rrd[o]], [Rya[sl]])
                if i == 3:
                    cols = slice(c * 512, (c + 1) * 512)
                    self.dma('pool', self.YT[g * 256:(g + 1) * 256, cols].rearrange("(h d) t -> d h t", d=64), ya[:, sl],
                             [Rya[sl]], (), owner=Rya[sl])

            def stage1(q):
                h, c, j, nj = jobs[q]
                if h < 0:
                    return swa1(q)
                hs = h % 2
                ci = chunk_idx[(h, c)]
                sl = ci % 2
                if c == 0 and j == 0:
                    self.dma('sp', kt[:, hs, T:], self.KcT[h], (), [Rkt[hs]], owner=Rkt[hs])
                    self.dma('sp', kt[:, hs, 0:T], self.KcTg[h // 2, (h % 2) * 66:(h % 2) * 66 + 66, :], (), [Rkt[hs]], owner=Rkt[hs])
                    self.dma('sp', vca[:, hs, NT:, 0:64], self.Vc[:, h * 64:(h + 1) * 64].rearrange("(j p) d -> p j d", p=128),
                             (), [Rvca[hs]], owner=Rvca[hs])
                    self.dma('sp', vca[:, hs, 0:NT, 0:64], self.Vcg[0:T, h * 64:(h + 1) * 64].rearrange("(j p) d -> p j d", p=128),
                             (), [Rvca[hs]], owner=Rvca[hs])
                if j == 0:
                    cols = slice(c * 512, (c + 1) * 512)
                    self.dma('sp', qa[:, sl, :], self.QcT[h, :, cols], (), [Rqa[sl]], owner=Rqa[sl])
                    self.ts('dve', bt[:, sl, NT:nj], self.cumK[:, 0:nj - NT, h], -1.0, self.Cbc[:, c, h:h + 1], ALU.mult, ALU.add,
                            [self.R_cum], [Rbt[sl]])
                    self.tt('dve', cq[:, sl, :], self.Cbc[:, c, h:h + 1], poff[:, h:h + 1], ALU.add, [self.R_cum, Rcp], [Rcq[sl]])
                    self.ts('dve', bt[:, sl, 0:NT], cpv[:, :, h], -1.0, cq[:, sl, :], ALU.mult, ALU.add,
                            [Rcp, Rcq[sl]], [Rbt[sl]])
                b = q % 3
                diag = j >= NT + 4 * c
                self.mm(ps[b][:], kt[:, hs, j * 128:(j + 1) * 128], qa[:, sl, :], True, not diag, [Rkt[hs], Rqa[sl]], [Rps[b]])
                if diag:
                    self.mm(ps[b][:], self.ident_b[:], fmask[:, j - NT - 4 * c, :], False, True, [RC, Rm], [Rps[b]])

            def stage2(q):
                h, c, j, nj = jobs[q]
                if h < 0:
                    return swa2(q)
                hs = h % 2
                ci = chunk_idx[(h, c)]
                sl = ci % 2
                o = ci % 2
                b = q % 3
                self.act(pt[:, b, :], ps[b][:], AF.Exp, [Rps[b], Rbt[sl]], [Rpt[b]], bias=bt[:, sl, j:j + 1])
                self.mm(po[o][0:65, :], vca[:, hs, j, 0:65], pt[:, b, :], j == 0, j == nj - 1, [Rvca[hs], Rpt[b]], [Rpo[o]])

            def stage3(q):
                h, c, j, nj = jobs[q]
                if h < 0:
                    return swa3(q)
                if j != nj - 1:
                    return
                ci = chunk_idx[(h, c)]
                sl = ci % 2
                o = ci % 2
                cols = slice(c * 512, (c + 1) * 512)
                self._normalize(po[o], Rpo[o], pb[o], Rpb[o], rd[:, o], Rrd[o], osb[:, o], Rosb[o],
                                yc[:, sl, :], Ryc[sl])
                self.dma('pool', self.YT[768 + h * 64:768 + (h + 1) * 64, cols], yc[:, sl, :], [Ryc[sl]], (), owner=Ryc[sl])
            if getattr(self, 'verbose', False):
                print("fox sbuf remaining", nc.sbuf_bytes_remaining)
            NJ = len(jobs)
            DEF = 2
            STEP = max(1, (NJ - 8) // (len(side) + 1))
            si = 0
            for q in range(NJ + LA + DEF):
                if 0 <= q - LA - DEF < NJ:
                    stage3(q - LA - DEF)
                if q < NJ:
                    stage1(q)
                if 0 <= q - LA < NJ:
                    stage2(q - LA)
                if q % STEP == STEP - 1 and si < len(side):
                    side[si](); si += 1
            while si < len(side):
                side[si](); si += 1
            exh, Rexh = self.lru_exh
            self.dma('sp', self.EXH, exh[:], [Rexh], (), owner=Rexh)
            self.allgather(self.EXH, self.EXHg, "cc_h")
            s.barrier()
            s.flush()

    def phase_moe(self, l, last):
        nc, s = self.nc, self.s
        T, NCH, NT = self.T, self.NCH, self.NT
        RC, RM = self.R_const, self.R_mod
        xsrc = self.x_in if l == 0 else self.xs
        with ExitStack() as ph:
            def A(name, shape, dtype):
                return ph.enter_context(nc.sbuf_tensor(self.un(name), list(shape), dtype))

            def P(name, shape, dtype):
                return ph.enter_context(nc.psum_tensor(self.un(name), list(shape), dtype))
            woutb = A("woutb", [128, 8, D], BF16); Rwo = s.res("woutb")
            self.dma('sp', woutb[:], self.WO, (), [Rwo], owner=Rwo)
            wr = A("wr", [128, 8, 20], F32); brb = A("brb", [128, 20], F32); Rwr = s.res("wr")
            self.dma('sp', wr[:, :, 0:4], self.w_rg[l].rearrange("(kc p) n -> p kc n", p=128), (), [Rwr], owner=Rwr)
            self.dma('sp', wr[:, :, 4:20], self.w_re[l].rearrange("(kc p) n -> p kc n", p=128), (), [Rwr], owner=Rwr)
            self.dma('sp', brb[:, 0:4], self.b_rg[l].partition_broadcast(128), (), [Rwr], owner=Rwr)
            self.dma('sp', brb[:, 4:20], self.b_re[l].partition_broadcast(128), (), [Rwr], owner=Rwr)
            invw = A("invw", [128, 3], F32)
            self.memset('pool', invw[:, 0:1], 1.0 / 512, [Rwr])
            self.memset('pool', invw[:, 1:3], 1.0 / 256, [Rwr])
            sel = A("sel", [16, 16, 128], BF16)
            self.cp('dve', sel[:], self.ident_f[0:16, 0:16].unsqueeze(2).to_broadcast([16, 16, 128]), [RC], [Rwr])
            h0 = A("h0", [128, 2], F32); Rh0 = s.res("h0")
            self.dma('sp', self.shm[:], self.MODS[l, :, 5 * D:6 * D], (), [RM], owner=RM)
            self.dma('sp', h0[:], self.EXHg[0:128, 0:2], (), [Rh0], owner=Rh0)
            self.ts('dve', h0[:], h0[:], self.flg[:, 0:1], None, ALU.mult, None, [Rh0, RC], [Rh0])
            yT = A("yT", [128, 8, 512], BF16); RyT = s.res("yT")
            zt = A("zt", [128, 2, 512], BF16); Rzt = s.res("zt")
            sqy = A("sqy", [128, 2, 8, 128], BF16); Rsqy = [s.res("sqy0"), s.res("sqy1")]
            x1 = A("x1", [128, 2, 4, D], F32); Rx1 = [s.res("x10"), s.res("x11")]
            rs = A("rs", [128, 2, 12], F32); Rrs = [s.res("rs0"), s.res("rs1")]
            junk = A("junk", [128, D], BF16); Rjunk = s.res("junk")
            st = A("st", [128, 2, 4], F32); Rst = [s.res("st0"), s.res("st1")]
            u2 = A("u2", [128, 1, D], F32); Ru2 = [s.res("u20")] * 2
            h2 = A("h2", [128, 1, D], F32); Rh2 = [s.res("h20")] * 2
            h2T32 = A("h2T32", [128, 1, 8, 128], F32); Rh32 = [s.res("h2T320")] * 2
            h2T = A("h2T", [128, 2, 8, 512], BF16); Rh2T = [s.res("h2T0"), s.res("h2T1")]
            rtt = A("rtr", [128, 2, 96], F32); Rrtt = [s.res("rtr0"), s.res("rtr1")]
            combT = A("combT", [16, 2, 512], BF16); RcT = [s.res("combT0"), s.res("combT1")]
            wgu = A("wgu", [128, 3, 8, 512], BF16); Rwgu = [s.res(f"wgu{i}") for i in range(3)]
            t1 = A("t1", [128, 2, 512], F32); Rt1 = [s.res("t10"), s.res("t11")]
            t2 = A("t2", [128, 2, 512], F32); Rt2 = [s.res("t20"), s.res("t21")]
            hid = A("hid", [128, 32, 512], BF16); Rhid = s.res("hid")
            wd = A("wd", [128, 2, 8, 512], BF16); Rwd = [s.res("wd0"), s.res("wd1")]
            B = [P(f"B{i}", [128, 512], F32) for i in range(4)]; RB = [s.res(f"B{i}") for i in range(4)]
            C0 = P("C0", [128, 512], F32); RC0 = s.res("C0")
            F01 = P("F01", [128, 8, 128], F32); RF01 = s.res("F01")
            F2 = P("F2", [128, 512], F32); RF2 = s.res("F2")
            F01f = F01[:].rearrange("p a b -> p (a b)")
            GR = [(0, [0, 1, 2, 3]), (1, [4, 5]), (2, [6, 7])]
            WB = [(F01f[:, 0:512], RF01), (F01f[:, 512:1024], RF01), (F2[:], RF2)]

            def front_pieces(c):
                sl = c % 2
                cols = slice(c * 512, (c + 1) * 512)
                X = x1[:, sl]
                RX = Rx1[sl]
                pcs = []

                def p_load():
                    self.dma('sp', yT[:], self.YT[:, cols].rearrange("(fc p) t -> p fc t", p=128), (), [RyT], owner=RyT)
                    self.dma('sp', zt[:], self.ZT[:, cols].rearrange("(k p) t -> p k t", p=128), (), [Rzt], owner=Rzt)
                    self.dma('sp', X, xsrc[c * 512:(c + 1) * 512, :].rearrange("(i p) d -> p i d", p=128), (), [RX], owner=RX)
                pcs.append(p_load)

                def p_corr():
                    for k in range(2):
                        self.stt(yT[:, 4 + k, :], zt[:, k, :], h0[:, k:k + 1], yT[:, 4 + k, :], ALU.mult, ALU.add, [Rzt, Rh0, RyT], [RyT])
                pcs.append(p_corr)

                def p_sq(i):
                    self.act(sqy[:, i % 2], yT[:, :, i * 128:(i + 1) * 128], AF.Square, [RyT], [Rsqy[i % 2]])

                def p_ss(i):
                    for g, fcs in GR:
                        for k_, fc in enumerate(fcs):
                            self.mm(F2[:, 256 + i * 3 + g:256 + i * 3 + g + 1], sqy[:, i % 2, fc, :], self.ones_b[:, 0:1],
                                    k_ == 0, k_ == len(fcs) - 1, [Rsqy[i % 2], RC], [RF2])
                pcs.append(lambda: p_sq(0))
                for i in range(4):
                    pcs.append(lambda i=i: (p_ss(i), p_sq(i + 1) if i < 3 else None))

                def p_rs():
                    r = rs[:, sl]
                    self.tt('dve', r.rearrange("p (a b) -> p a b", a=4), F2[:, 256:268].rearrange("p (a b) -> p a b", a=4),
                            invw[:].unsqueeze(1).to_broadcast([128, 4, 3]), ALU.mult, [RF2, Rwr], [Rrs[sl]])
                    self.ts('dve', r, r, EPS, None, ALU.add, None, [Rrs[sl]], [Rrs[sl]])
                    self.act(r, r, AF.Sqrt, [Rrs[sl]], [Rrs[sl]])
                    self.recip(r, r, [Rrs[sl]], [Rrs[sl]])
                pcs.append(p_rs)

                def p_wout(i, nh):
                    for g, fcs in GR:
                        bank, Rb = WB[g]
                        for k_, fc in enumerate(fcs):
                            self.mm(bank, yT[:, fc, i * 128:(i + 1) * 128], woutb[:, fc, nh * 512:(nh + 1) * 512],
                                    k_ == 0, k_ == len(fcs) - 1, [RyT, Rwo], [Rb])
                    for g, _ in GR:
                        bank, Rb = WB[g]
                        xs_ = X[:, i, nh * 512:(nh + 1) * 512]
                        self.stt(xs_, bank, rs[:, sl, i * 3 + g:i * 3 + g + 1], xs_, ALU.mult, ALU.add, [Rb, Rrs[sl], RX], [RX])
                for i in range(4):
                    for nh in range(2):
                        pcs.append(lambda i=i, nh=nh: p_wout(i, nh))

                def p_norm(i):
                    q = i % 2
                    self.act(junk[:], X[:, i, :], AF.Square, [RX], [Rjunk, Rst[q]], accum_out=st[:, q, 0:1])
                    self.ts('dve', st[:, q, 1:2], st[:, q, 0:1], 1.0 / D, EPS, ALU.mult, ALU.add, [Rst[q]], [Rst[q]])
                    self.act(st[:, q, 2:3], st[:, q, 1:2], AF.Sqrt, [Rst[q]], [Rst[q]])
                    self.recip(st[:, q, 3:4], st[:, q, 2:3], [Rst[q]], [Rst[q]])
                    self.stt(u2[:, 0], X[:, i, :], st[:, q, 3:4], self.A_ffn[:], ALU.mult, ALU.mult, [RX, Rst[q], RM], [Ru2[q]])
                    self.tt('pool', h2[:, 0], u2[:, 0], self.shf[:], ALU.add, [Ru2[q], RM], [Rh2[q]])

                def p_tr(i):
                    q = i % 2
                    for dc in range(8):
                        self.tr(F01[:, dc, :], h2[:, 0, dc * 128:(dc + 1) * 128], self.ident_f[:], [Rh2[q], RC], [RF01])
                    self.cp('act', h2T32[:, 0], F01[:], [RF01], [Rh32[q]])
                    self.cp('pool', h2T[:, sl, :, i * 128:(i + 1) * 128], h2T32[:, 0], [Rh32[q]], [Rh2T[sl]])

                def p_route(i):
                    q = i % 2
                    rt = rtt[:, q]
                    for dc in range(8):
                        self.mm(F2[:, 0:20], h2T32[:, 0, dc, :], wr[:, dc, :], dc == 0, dc == 7, [Rh32[q], Rwr], [RF2])
                    lg = rt[:, 0:20]; gl = rt[:, 0:4]; el3 = rt[:, 4:20].rearrange("p (g i) -> p g i", g=4)
                    gmax = rt[:, 20:21]; ngmax = rt[:, 21:22]; goh = rt[:, 22:26]; ge = rt[:, 26:30]; gsum = rt[:, 30:31]
                    tmp3 = rt[:, 32:48].rearrange("p (g i) -> p g i", g=4); ing = rt[:, 48:52]
                    m1 = rt[:, 52:53]; nm1 = rt[:, 53:54]; oh1 = rt[:, 54:58]; msk = rt[:, 58:62]; m2 = rt[:, 62:63]
                    selm = rt[:, 64:68]; ee = rt[:, 68:72]; es = rt[:, 72:76]; ssum = rt[:, 76:77]; fac = rt[:, 77:78]
                    comb = rt[:, 80:96]
                    RR = [Rrtt[q]]
                    self.tt('dve', lg, F2[:, 0:20], brb[:], ALU.add, [RF2, Rwr], RR)
                    s.op('dve', lambda e: e.tensor_reduce(out=gmax, in_=gl, axis=AX.X, op=ALU.max), RR, RR)
                    self.ts('dve', goh, gl, gmax, None, ALU.is_equal, None, RR, RR)
                    self.ts('dve', ngmax, gmax, -1.0, None, ALU.mult, None, RR, RR)
                    self.act(ge, gl, AF.Exp, RR, RR, bias=ngmax, accum_out=gsum)
                    self.tt('dve', tmp3, el3, goh.unsqueeze(2).to_broadcast([128, 4, 4]), ALU.mult, RR, RR)
                    s.op('dve', lambda e: e.tensor_reduce(out=ing, in_=tmp3.rearrange("p g i -> p i g"), axis=AX.X, op=ALU.add), RR, RR)
                    s.op('dve', lambda e: e.tensor_reduce(out=m1, in_=ing, axis=AX.X, op=ALU.max), RR, RR)
                    self.ts('dve', oh1, ing, m1, None, ALU.is_equal, None, RR, RR)
                    self.stt(msk, oh1, -1e30, ing, ALU.mult, ALU.add, RR, RR)
                    s.op('dve', lambda e: e.tensor_reduce(out=m2, in_=msk, axis=AX.X, op=ALU.max), RR, RR)
                    self.ts('dve', selm, ing, m2, None, ALU.is_ge, None, RR, RR)
                    self.ts('dve', nm1, m1, -1.0, None, ALU.mult, None, RR, RR)
                    self.act(ee, ing, AF.Exp, RR, RR, bias=nm1)
                    self.tt('dve', es, ee, selm, ALU.mult, RR, RR)
                    s.op('dve', lambda e: e.tensor_reduce(out=ssum, in_=es, axis=AX.X, op=ALU.add), RR, RR)
                    self.tt('dve', fac, ssum, gsum, ALU.mult, RR, RR)
                    self.recip(fac, fac, RR, RR)
                    self.ts('dve', es, es, fac, None, ALU.mult, None, RR, RR)
                    self.tt('dve', comb.rearrange("p (g i) -> p g i", g=4), goh.unsqueeze(2).to_broadcast([128, 4, 4]),
                            es.unsqueeze(1).to_broadcast([128, 4, 4]), ALU.mult, RR, RR)

                def p_comb(i):
                    q = i % 2
                    self.tr(F2[0:16, 128:256], rtt[:, q, 80:96], self.ident_f[:], [Rrtt[q], RC], [RF2])
                    self.cp('dve', combT[:, sl, i * 128:(i + 1) * 128], F2[0:16, 128:256], [RF2], [RcT[sl]])
                for step in range(7):
                    def piece(step=step):
                        if 0 <= step - 3 < 4:
                            p_comb(step - 3)
                        if 0 <= step - 2 < 4:
                            p_route(step - 2)
                        if 0 <= step - 1 < 4:
                            p_tr(step - 1)
                        if step < 4:
                            p_norm(step)
                    pcs.append(piece)
                return pcs

            for p in front_pieces(0):
                p()
            ge_ = 0
            gw = 0

            def load_wgu(n):
                e = n % 16
                sl_ = n % 3
                self.dma('sp', wgu[:, sl_], self.WGU[e], (), [Rwgu[sl_]], owner=Rwgu[sl_])

            def load_wd(n):
                g_ = n % 8
                ws = n % 2
                self.dma('sp', wd[:, ws], self.WD[g_ // 4, g_ % 4], (), [Rwd[ws]], owner=Rwd[ws])
            tot_e = 16 * NCH
            tot_w = 8 * NCH
            for n in range(3):
                load_wgu(n)
            for c in range(NCH):
                sl = c % 2
                X = x1[:, sl]
                RX = Rx1[sl]
                pcs = front_pieces(c + 1) if c + 1 < NCH else []
                NSLOT = 24
                pi = 0

                def emit_pieces(slot):
                    nonlocal pi
                    tgt = (len(pcs) * (slot + 1)) // NSLOT
                    while pi < tgt:
                        pcs[pi]()
                        pi += 1
                for e in range(16):
                    n = c * 16 + e
                    ws_ = n % 3
                    for k in range(2):
                        pg, pu = (0, 1) if k == 0 else (2, 3)
                        for dc in range(8):
                            self.mm(B[pg][:], wgu[:, ws_, dc, k * 128:(k + 1) * 128], h2T[:, sl, dc, :], dc == 0, dc == 7,
                                    [Rwgu[ws_], Rh2T[sl]], [RB[pg]])
                        for dc in range(8):
                            self.mm(B[pu][:], wgu[:, ws_, dc, 256 + k * 128:256 + (k + 1) * 128], h2T[:, sl, dc, :], dc == 0, dc == 7,
                                    [Rwgu[ws_], Rh2T[sl]], [RB[pu]])
                        if k == 0:
                            self.mm(C0[:], sel[:, e, :], combT[:, sl, :], True, True, [Rwr, RcT[sl]], [RC0])
                        self.act(t1[:, k, :], B[pg][:], AF.Silu, [RB[pg]], [Rt1[k]])
                        self.tt('dve', t2[:, k, :], t1[:, k, :], B[pu][:], ALU.mult, [Rt1[k], RB[pu]], [Rt2[k]])
                        self.tt('dve', hid[:, e * 2 + k, :], t2[:, k, :], C0[:], ALU.mult, [Rt2[k], RC0], [Rhid])
                    if n + 3 < tot_e:
                        load_wgu(n + 3)
                    if e == 13:
                        load_wd(c * 8)
                    if e == 14:
                        load_wd(c * 8 + 1)
                    emit_pieces(e)
                for nh in range(2):
                    for eq in range(4):
                        n = c * 8 + nh * 4 + eq
                        ws = n % 2
                        for i in range(4):
                            for ek in range(8):
                                self.mm(B[i][:], hid[:, eq * 8 + ek, i * 128:(i + 1) * 128], wd[:, ws, ek, :],
                                        eq == 0 and ek == 0, eq == 3 and ek == 7, [Rhid, Rwd[ws]], [RB[i]])
                        if n + 2 < tot_w and not (nh == 1 and eq >= 2):
                            load_wd(n + 2)
                        emit_pieces(16 + nh * 4 + eq)
                    for i in range(4):
                        xs_ = X[:, i, nh * 512:(nh + 1) * 512]
                        self.tt('dve', t1[:, i % 2, :], B[i][:], self.shm[:, nh * 512:(nh + 1) * 512], ALU.mult, [RB[i], RM], [Rt1[i % 2]])
                        self.tt('dve', xs_, xs_, t1[:, i % 2, :], ALU.add, [RX, Rt1[i % 2]], [RX])
                if not last:
                    self.dma('pool', self.xs[c * 512:(c + 1) * 512, :].rearrange("(i p) d -> p i d", p=128), X, [RX], (), owner=RX)
                else:
                    for i in range(4):
                        q = i % 2
                        self.act(junk[:], X[:, i, :], AF.Square, [RX], [Rjunk, Rst[q]], accum_out=st[:, q, 0:1])
                        self.ts('dve', st[:, q, 1:2], st[:, q, 0:1], 1.0 / D, EPS, ALU.mult, ALU.add, [Rst[q]], [Rst[q]])
                        self.act(st[:, q, 2:3], st[:, q, 1:2], AF.Sqrt, [Rst[q]], [Rst[q]])
                        self.recip(st[:, q, 3:4], st[:, q, 2:3], [Rst[q]], [Rst[q]])
                        self.stt(X[:, i, :], X[:, i, :], st[:, q, 3:4], self.nfin[:], ALU.mult, ALU.mult, [RX, Rst[q], RC], [RX])
                    self.dma('pool', self.out[c * 512:(c + 1) * 512, :].rearrange("(i p) d -> p i d", p=128), X, [RX], (), owner=RX)
            s.barrier()
            s.flush()


def host_inputs(inputs, T=T_FULL):
    f = lambda a: np.ascontiguousarray(np.asarray(a, dtype=np.float32))
    L = L_FULL
    com = {
        "w_mod": f(inputs["w_mod"]), "b_mod": f(inputs["b_mod"]),
        "norm_mix": f(inputs["norm_mix"]), "norm_ffn": f(inputs["norm_ffn"]),
        "w_in": f(inputs["w_in"]), "w_out": f(inputs["w_out"]),
        "gainT": f(np.asarray(inputs["out_gain"]).reshape(L, 8, 128).transpose(0, 2, 1)),
        "sinks": f(inputs["sinks"]),
        "convT": f(np.asarray(inputs["conv_w"]).transpose(0, 2, 1).reshape(L, 2, 128, 4).transpose(0, 2, 1, 3)),
        "convb": f(np.asarray(inputs["conv_b"]).reshape(L, 2, 128).transpose(0, 2, 1)),
        "lru_wa": f(inputs["lru_wa"]), "lru_wx": f(inputs["lru_wx"]),
        "lru_ba": f(np.asarray(inputs["lru_ba"]).reshape(L, 2, 128).transpose(0, 2, 1)),
        "lru_bx": f(np.asarray(inputs["lru_bx"]).reshape(L, 2, 128).transpose(0, 2, 1)),
        "lru_lam": f(np.asarray(inputs["lru_lam"]).reshape(L, 2, 128).transpose(0, 2, 1)),
        "fox_bf": f(inputs["fox_bf"]),
        "w_rg": f(inputs["w_router_group"]), "b_rg": f(inputs["b_router_group"]),
        "w_re": f(inputs["w_router_expert"]), "b_re": f(inputs["b_router_expert"]),
        "w_gate": f(inputs["w_gate"]), "w_up": f(inputs["w_up"]), "w_down": f(inputs["w_down"]),
        "norm_final": f(inputs["norm_final"]),
    }
    x = np.asarray(inputs["x"], dtype=np.float32)
    c = np.asarray(inputs["c"], dtype=np.float32)
    maps = []
    for core in range(8):
        b, r = core // 2, core % 2
        m = dict(com)
        m["x"] = np.ascontiguousarray(x[b, r * T:(r + 1) * T])
        m["cT"] = np.ascontiguousarray(c[b].reshape(8, 128).T)
        fl = np.zeros((128, 4), np.float32)
        fl[:, 0] = float(r)
        fl[:, 1] = 0.0 if r else NEG
        m["flags"] = fl
        maps.append(m)
    return maps


_CACHE = {}


def kernel(**inputs):
    if "nc" not in _CACHE:
        _CACHE["nc"] = K().build()
    nc = _CACHE["nc"]
    maps = host_inputs(inputs)
    res = run_bass_kernel_spmd(nc, maps, core_ids=list(range(8)))
    out = np.stack([np.concatenate([np.asarray(res.results[2 * b + r]["out"], dtype=np.float32) for r in range(2)], axis=0)
                    for b in range(4)], axis=0)
    return out
```

```python
import numpy as np
from contextlib import ExitStack
import concourse.bass as bass
import concourse.mybir as mybir
from concourse.bass_utils import run_bass_kernel_spmd

F32 = mybir.dt.float32
BF16 = mybir.dt.bfloat16
AF = mybir.ActivationFunctionType
ALU = mybir.AluOpType
AX = mybir.AxisListType

D = 1024
L_FULL = 2
T_FULL = 4096
IN_COLS = 2052
EPS = 1e-6
NEG = -30000.0
COMPUTE = ('pe', 'act', 'dve', 'pool')
ENGS = ('pe', 'act', 'dve', 'pool', 'sp')
BLKNAME = {'pe': 'tensor', 'act': 'scalar', 'dve': 'vector', 'pool': 'gpsimd', 'sp': 'sync'}


class Res:
    __slots__ = ('name', 'lw', 'rd', 'sem', 'semcnt')

    def __init__(self, name):
        self.name = name
        self.lw = None
        self.rd = {}
        self.sem = None
        self.semcnt = 0


class Sched:
    BLK = 8192
    NS = 4

    def __init__(self, nc, es):
        self.nc = nc
        self.es = es
        self.q = {e: [] for e in ENGS}
        self.cnt = {e: 0 for e in COMPUTE}
        self.esem = {e: [es.enter_context(nc.semaphore(f"s_{e}{i}")) for i in range(self.NS)] for e in COMPUTE}
        self.waited = {e: {} for e in ENGS}
        self.pending = {e: None for e in ENGS}
        self.dma_res = []
        self.nsem = 0
        self.nops = 0
        self.rescache = {}

    def res(self, name):
        if name not in self.rescache:
            self.rescache[name] = Res(name)
        return self.rescache[name]

    def _semval(self, src, idx):
        s = self.esem[src][(idx // self.BLK) % self.NS]
        v = (idx // (self.BLK * self.NS)) * self.BLK + (idx % self.BLK) + 1
        return s, v

    def op(self, eng, fn, reads=(), writes=(), dma=None, inc=16):
        deps = set()
        for r in reads:
            if r.lw is not None:
                deps.add(r.lw)
        for w in writes:
            if w.lw is not None:
                deps.add(w.lw)
            for h in w.rd.values():
                deps.add(h)
        if self.pending[eng] is not None:
            deps |= self.pending[eng]
            self.pending[eng] = None
        if dma is not None:
            if dma.sem is None:
                dma.sem = self.es.enter_context(self.nc.semaphore(f"d_{self.nsem}"))
                self.nsem += 1
                self.dma_res.append(dma)
            dma.semcnt += inc
            h = ('d', dma, dma.semcnt, inc)
        else:
            assert eng in COMPUTE
            h = ('c', eng, self.cnt[eng])
            self.cnt[eng] += 1
        self.q[eng].append((fn, deps, h))
        self.nops += 1
        for r in reads:
            key = h[1]
            r.rd[key] = h
        for w in writes:
            w.lw = h
            w.rd = {}
        return h

    def barrier(self):
        hs = set()
        for e in COMPUTE:
            if self.cnt[e] > 0:
                hs.add(('c', e, self.cnt[e] - 1))
        for r in self.dma_res:
            if r.semcnt > 0:
                hs.add(('d', r, r.semcnt, 0))
        for e in ENGS:
            self.pending[e] = set(hs) if self.pending[e] is None else (self.pending[e] | hs)

    def _emit(self, ename, eng, ops):
        waited = self.waited[ename]
        for fn, deps, h in ops:
            for d in deps:
                if d[0] == 'c':
                    _, src, idx = d
                    if src == ename and ename == 'pe':
                        continue
                    if waited.get(src, -1) >= idx:
                        continue
                    waited[src] = idx
                    sm, v = self._semval(src, idx)
                    eng.wait_ge(sm, v)
                else:
                    r, c = d[1], d[2]
                    if waited.get(r, -1) >= c:
                        continue
                    waited[r] = c
                    eng.wait_ge(r.sem, c)
            ins = fn(eng)
            if h[0] == 'c':
                sm, v = self._semval(h[1], h[2])
                ins.then_inc(sm, 1)
            elif h[3] == 16:
                ins.then_inc(h[1].sem, 16)
            else:
                ins.then_inc(h[1].sem)

    def flush(self):
        if not any(self.q[e] for e in ENGS):
            return
        with self.nc.Block() as blk:
            for e in ENGS:
                ops = self.q[e]
                if not ops:
                    continue

                def body(eng, e=e, ops=ops):
                    self._emit(e, eng, ops)
                getattr(blk, BLKNAME[e])(body)
        for e in ENGS:
            self.q[e] = []

    def finish(self):
        self.barrier()
        deps = self.pending['sp']
        self.pending['sp'] = None
        with self.nc.Block() as blk:
            def body(eng):
                for d in deps:
                    if d[0] == 'c':
                        sm, v = self._semval(d[1], d[2])
                        eng.wait_ge(sm, v)
                    else:
                        eng.wait_ge(d[1].sem, d[2])
            blk.sync(body)


class K:
    def __init__(self, T=T_FULL, L=L_FULL, dbg=False, stop_after=None):
        self.T, self.L, self.dbg = T, L, dbg
        self.stop_after = stop_after
        self.NT = T // 128
        self.NCH = T // 512
        self.rr = 0

    def un(self, name):
        self.uid = getattr(self, 'uid', 0) + 1
        return f"{name}_{self.uid}"

    def dma(self, q, out, in_, reads=(), writes=(), owner=None, **kw):
        return self.s.op(q, lambda e: e.dma_start(out=out, in_=in_, **kw), reads, writes, dma=owner)

    def allgather(self, src, dst, name):
        R = self.s.res(name)
        self.s.barrier()
        h = self.s.op('pool', lambda e: e.collective_compute("AllGather", ALU.bypass,
                                                            replica_groups=[[0, 1], [2, 3], [4, 5], [6, 7]],
                                                            ins=[src.opt()], outs=[dst.opt()]), (), (), dma=R, inc=1)
        return h

    def mm(self, out, lhsT, rhs, start, stop, reads, writes):
        return self.s.op('pe', lambda e: e.matmul(out, lhsT, rhs, start=start, stop=stop), reads, writes)

    def tr(self, out, in_, ident, reads, writes):
        return self.s.op('pe', lambda e: e.transpose(out, in_, ident), reads, writes)

    def act(self, out, in_, func, reads, writes, bias=None, scale=None, accum_out=None, eng='act'):
        kw = {}
        if bias is not None:
            kw['bias'] = bias
        if scale is not None:
            kw['scale'] = scale
        if accum_out is not None:
            kw['accum_out'] = accum_out
        return self.s.op('act', lambda e: e.activation(out=out, in_=in_, func=func, **kw), reads, writes)

    def ts(self, eng, out, in0, s1, s2, op0, op1, reads, writes, accum_out=None):
        if op1 is None:
            return self.s.op(eng, lambda e: e.tensor_scalar(out=out, in0=in0, scalar1=s1, scalar2=None, op0=op0), reads, writes)
        return self.s.op(eng, lambda e: e.tensor_scalar(out=out, in0=in0, scalar1=s1, scalar2=s2, op0=op0, op1=op1), reads, writes)

    def tt(self, eng, out, in0, in1, op, reads, writes):
        return self.s.op(eng, lambda e: e.tensor_tensor(out=out, in0=in0, in1=in1, op=op), reads, writes)

    def stt(self, out, in0, scalar, in1, op0, op1, reads, writes):
        return self.s.op('dve', lambda e: e.scalar_tensor_tensor(out=out, in0=in0, scalar=scalar, in1=in1, op0=op0, op1=op1), reads, writes)

    def cp(self, eng, out, in_, reads, writes):
        if eng == 'act':
            return self.s.op('act', lambda e: e.copy(out=out, in_=in_), reads, writes)
        return self.s.op(eng, lambda e: e.tensor_copy(out=out, in_=in_), reads, writes)

    def memset(self, eng, ap, val, writes):
        return self.s.op(eng, lambda e: e.memset(ap, val), (), writes)

    def recip(self, out, in_, reads, writes):
        return self.s.op('dve', lambda e: e.reciprocal(out=out, in_=in_), reads, writes)

    def scan(self, out, d0, d1, init, op0, op1, reads, writes):
        return self.s.op('dve', lambda e: e.tensor_tensor_scan(out=out, data0=d0, data1=d1, initial=init, op0=op0, op1=op1), reads, writes)

    def asel(self, out, in_, pattern, cmp, fill, base, cm, reads, writes):
        return self.s.op('pool', lambda e: e.affine_select(out=out, in_=in_, pattern=pattern, compare_op=cmp, fill=fill, base=base, channel_multiplier=cm), reads, writes)

    def evac_eng(self):
        self.rr += 1
        return 'act' if self.rr % 2 else 'dve'

    def evac(self, out, in_, reads, writes, scale=None, eng=None):
        eng = eng or self.evac_eng()
        if eng == 'act':
            if scale is None:
                return self.s.op('act', lambda e: e.copy(out=out, in_=in_), reads, writes)
            return self.s.op('act', lambda e: e.activation(out=out, in_=in_, func=AF.Copy, scale=scale), reads, writes)
        if scale is None:
            return self.s.op('dve', lambda e: e.tensor_copy(out=out, in_=in_), reads, writes)
        return self.s.op('dve', lambda e: e.tensor_scalar(out=out, in0=in_, scalar1=scale, scalar2=None, op0=ALU.mult), reads, writes)

    def build(self):
        T, L = self.T, self.L
        nc = bass.Bass("TRN2", target_bir_lowering=False)
        self.nc = nc
        dt = nc.dram_tensor

        def inp(name, shape, dtype=F32):
            return dt(name, list(shape), dtype, kind="ExternalInput").ap()
        self.x_in = inp("x", [T, D])
        self.cT = inp("cT", [128, 8])
        self.w_mod = inp("w_mod", [L_FULL, D, 6 * D])
        self.b_mod = inp("b_mod", [L_FULL, 6 * D])
        self.norm_mix = inp("norm_mix", [L_FULL, D])
        self.norm_ffn = inp("norm_ffn", [L_FULL, D])
        self.w_in = inp("w_in", [L_FULL, D, IN_COLS])
        self.w_out = inp("w_out", [L_FULL, D, D])
        self.gainT = inp("gainT", [L_FULL, 128, 8])
        self.sinks = inp("sinks", [L_FULL, 8])
        self.convT = inp("convT", [L_FULL, 128, 2, 4])
        self.convb = inp("convb", [L_FULL, 128, 2])
        self.lru_wa = inp("lru_wa", [L_FULL, 8, 32, 32])
        self.lru_ba = inp("lru_ba", [L_FULL, 128, 2])
        self.lru_wx = inp("lru_wx", [L_FULL, 8, 32, 32])
        self.lru_bx = inp("lru_bx", [L_FULL, 128, 2])
        self.lru_lam = inp("lru_lam", [L_FULL, 128, 2])
        self.fox_bf = inp("fox_bf", [L_FULL, 4])
        self.w_rg = inp("w_rg", [L_FULL, D, 4])
        self.b_rg = inp("b_rg", [L_FULL, 4])
        self.w_re = inp("w_re", [L_FULL, D, 16])
        self.b_re = inp("b_re", [L_FULL, 16])
        self.w_gate = inp("w_gate", [L_FULL, 16, D, 256])
        self.w_up = inp("w_up", [L_FULL, 16, D, 256])
        self.w_down = inp("w_down", [L_FULL, 16, 256, D])
        self.norm_final = inp("norm_final", [D])
        self.flags = inp("flags", [128, 4])
        self.out = dt("out", [T, D], F32, kind="ExternalOutput").ap()
        sk = "ExternalOutput" if self.dbg else "Internal"

        def scr(name, shape, dtype):
            kind = "Internal" if name in ("KcT", "Vc", "EXC", "EXS", "EXH", "EXCg", "EXSg", "EXHg", "KcTg", "Vcg") else sk
            return dt(name, list(shape), dtype, kind=kind).ap()
        self.xs = scr("xs", [T, D], F32)
        self.QaT = scr("QaT", [512, T], BF16)
        self.KaT = scr("KaT", [128, T], BF16)
        self.Va = scr("Va", [T, 128], BF16)
        self.QcT = scr("QcT", [4, 66, T], BF16)
        self.KcT = scr("KcT", [4, 66, T], BF16)
        self.Vc = scr("Vc", [T, 256], BF16)
        self.YT = scr("YT", [D, T], BF16)
        NX = self.NT * 4 + 16
        self.NX = NX
        self.XB = scr("XB", [256, T], F32)
        self.GB = scr("GB", [256, T], F32)
        self.ZT = scr("ZT", [256, T], BF16)
        self.EXC = scr("EXC", [128, NX], F32)
        self.EXS = scr("EXS", [128, 256], BF16)
        self.EXH = scr("EXH", [128, 16], F32)
        self.EXCg = scr("EXCg", [256, NX], F32)
        self.EXSg = scr("EXSg", [256, 256], BF16)
        self.EXHg = scr("EXHg", [256, 16], F32)
        self.KcTg = scr("KcTg", [2, 264, T], BF16)
        self.Vcg = scr("Vcg", [2 * T, 256], BF16)
        self.WGU = scr("WGU", [16, 128, 8, 512], BF16)
        self.WO = scr("WO", [128, 8, D], BF16)
        self.MODS = scr("MODS", [L_FULL, 128, 6 * D], F32)
        self.WD = scr("WD", [2, 4, 128, 8, 512], BF16)
        if self.dbg:
            self.dbgmod = scr("dbgmod", [128, 6 * D], F32)
            self.dbgcum = scr("dbgcum", [128, self.NT, 4], F32)

        with ExitStack() as es:
            self.es = es
            self.s = Sched(nc, es)
            self.persistent()
            self.s.flush()
            for l in range(L):
                self.setup_layer(l)
                self.s.barrier(); self.s.flush()
                if self.stop_after == ('setup', l):
                    break
                self.phase_A(l)
                self.s.barrier(); self.s.flush()
                if self.stop_after == ('A', l):
                    break
                self.phase_fox(l)
                self.s.barrier(); self.s.flush()
                if self.stop_after == ('fox', l):
                    break
                self.phase_moe(l, last=(l == L - 1))
                self.s.barrier(); self.s.flush()
            self.s.finish()
        return nc

    def persistent(self):
        nc, es, s = self.nc, self.es, self.s

        def A(name, shape, dtype):
            return es.enter_context(nc.sbuf_tensor(self.un(name), list(shape), dtype))
        self.ident_f = A("ident_f", [128, 128], F32)
        self.ident_b = A("ident_b", [128, 128], BF16)
        self.ones_f = A("ones_f", [128, 128], F32)
        self.ones_b = A("ones_b", [128, 512], BF16)
        self.shm = A("shm", [128, D], F32)
        self.shf = A("shf", [128, D], F32)
        self.A_mix = A("A_mix", [128, D], F32)
        self.A_ffn = A("A_ffn", [128, D], F32)
        self.nfin = A("nfin", [128, D], F32)
        self.csil = A("csil", [128, 8], F32)
        self.exc = A("exc", [128, self.NT * 4 + 16], F32)
        self.cumK = self.exc[:, 0:self.NT * 4].rearrange("p (a b) -> p a b", b=4)
        self.flg = A("flg", [128, 4], F32)
        self.Cbc = A("Cbc", [128, self.NCH, 4], F32)
        self.R_const = s.res("const")
        self.R_mod = s.res("mod")
        self.R_cum = s.res("cum")
        R = self.R_const
        self.memset('pool', self.ones_f[:], 1.0, [R])
        self.memset('pool', self.ones_b[:], 1.0, [R])
        self.asel(self.ident_f[:], self.ones_f[:], [[-1, 128]], ALU.is_equal, 0.0, 0, 1, [R], [R])
        self.cp('pool', self.ident_b[:], self.ident_f[:], [R], [R])
        with ExitStack() as ph:
            ct = ph.enter_context(nc.sbuf_tensor(self.un("ct"), [128, 8], F32))
            Rt = s.res("tmp")
            self.dma('sp', ct[:], self.cT, (), [Rt], owner=Rt)
            self.act(self.csil[:], ct[:], AF.Silu, [Rt], [R])
            self.memset('pool', self.exc[:], 0.0, [self.R_cum])
            self.dma('sp', self.flg[:], self.flags, (), [Rt], owner=Rt)
            Rn = s.res("nfin")
            self.dma('sp', self.nfin[:], self.norm_final.partition_broadcast(128), (), [Rn], owner=Rn)
            for h in range(4):
                for c in range(self.NCH):
                    self.dma('sp', self.KcT[h, 64:66, c * 512:(c + 1) * 512], self.ones_b[0:2, :], [R], (), owner=Rt)
            s.barrier()
            s.flush()

    def mod_ops(self, l, mod, cB, wm, Rwm, pm, Rpm, R, q='sp'):
        pcs = []
        wv = self.w_mod[l].rearrange("(kc p) n -> p kc n", p=128)
        pcs.append(lambda: self.dma(q, mod[:], self.b_mod[l].partition_broadcast(128), (), [R], owner=R))
        for n in range(12):
            sl = n % 2
            pcs.append(lambda n=n, sl=sl: self.dma(q, wm[:, sl], wv[:, :, n * 512:(n + 1) * 512], (), [Rwm[sl]], owner=Rwm[sl]))

            def f(n=n, sl=sl):
                for kc in range(8):
                    self.mm(pm[:], cB[:, kc, :], wm[:, sl, kc, :], kc == 0, kc == 7, [R, Rwm[sl]], [Rpm])
            pcs.append(f)
            pcs.append(lambda n=n: self.tt('dve', mod[:, n * 512:(n + 1) * 512], pm[:], mod[:, n * 512:(n + 1) * 512],
                                           ALU.add, [Rpm, R], [R]))
        return pcs

    def mod_ahead_pieces(self, l, A, px, Rpx):
        s = self.s
        pcs = []
        cB = A("cBa", [128, 8, 128], F32); RcB = s.res("cBa")
        wm = A("wma", [128, 8, 256], F32); Rwm = s.res("wma")
        bm = A("bma", [128, 256], F32); Rbm = s.res("bma")
        wv = self.w_mod[l].rearrange("(kc p) n -> p kc n", p=128)
        pcs.append(lambda: self.cp('dve', cB[:], self.csil[:].unsqueeze(2).to_broadcast([128, 8, 128]), [self.R_const], [RcB]))
        for n in range(24):
            cs = slice(n * 256, (n + 1) * 256)

            def ld(n=n, cs=cs):
                self.dma('pool', wm[:], wv[:, :, cs], (), [Rwm], owner=Rwm)
                self.dma('pool', bm[:], self.b_mod[l, cs].partition_broadcast(128), (), [Rbm], owner=Rbm)
            pcs.append(ld)

            def f(n=n):
                for kc in range(8):
                    self.mm(px[:, 0:256], cB[:, kc, :], wm[:, kc, :], kc == 0, kc == 7, [RcB, Rwm], [Rpx])
                self.tt('dve', bm[:], px[:, 0:256], bm[:], ALU.add, [Rpx, Rbm], [Rbm])
            pcs.append(f)
            pcs.append(lambda n=n, cs=cs: self.dma('pool', self.MODS[l, :, cs], bm[:], [Rbm], (), owner=Rbm))
        return pcs

    def setup_layer(self, l):
        nc, s = self.nc, self.s
        R = self.R_mod
        with ExitStack() as ph:
            def A(name, shape, dtype):
                return ph.enter_context(nc.sbuf_tensor(self.un(name), list(shape), dtype))
            mod = A("mod", [128, 6 * D], F32)
            nb = A("nb", [128, 2, D], F32)
            Rnb = s.res("nb")
            self.dma('sp', nb[:, 0, :], self.norm_mix[l].partition_broadcast(128), (), [Rnb], owner=Rnb)
            self.dma('sp', nb[:, 1, :], self.norm_ffn[l].partition_broadcast(128), (), [Rnb], owner=Rnb)
            if l == 0:
                cB = A("cB", [128, 8, 128], F32)
                wm = A("wm", [128, 2, 8, 512], F32)
                Rwm = [s.res("wm0"), s.res("wm1")]
                pm = ph.enter_context(nc.psum_tensor(self.un("pm"), [128, 512], F32))
                Rpm = s.res("pm0")
                self.cp('dve', cB[:], self.csil[:].unsqueeze(2).to_broadcast([128, 8, 128]), [self.R_const], [R])
                for p in self.mod_ops(l, mod, cB, wm, Rwm, pm, Rpm, R):
                    p()
                self.dma('pool', self.MODS[l], mod[:], [R], (), owner=Rnb)
            else:
                self.dma('sp', mod[:], self.MODS[l], (), [R], owner=R)
            self.stt(self.A_mix[:], mod[:, D:2 * D], 1.0, nb[:, 0, :], ALU.add, ALU.mult, [R, Rnb], [R])
            self.stt(self.A_ffn[:], mod[:, 4 * D:5 * D], 1.0, nb[:, 1, :], ALU.add, ALU.mult, [R, Rnb], [R])
            self.cp('pool', self.shm[:], mod[:, 0:D], [R], [R])
            self.cp('pool', self.shf[:], mod[:, 3 * D:4 * D], [R], [R])
            if self.dbg and l == 0:
                self.dma('sp', self.dbgmod, mod[:], [R], (), owner=Rnb)
            s.barrier()
            s.flush()

    def conv_pieces(self, l, A):
        s = self.s
        pcs = []
        gm = A("gm", [128, D], F32); Rgm = s.res("gm")
        gain = A("gain", [128, 8], F32)
        wst = A("wst", [128, 2, D], F32); Rwst = [s.res("wst0"), s.res("wst1")]
        wob = A("wob", [128, D], BF16); Rwob = s.res("wob0")
        Rcv = s.res("cvdma")

        def p0():
            self.dma('pool', gm[:], self.MODS[l, :, 2 * D:3 * D], (), [Rgm], owner=Rgm)
            self.dma('pool', gain[:], self.gainT[l], (), [Rgm], owner=Rgm)
        pcs.append(p0)
        for fc in range(8):
            sl = fc % 2
            pcs.append(lambda fc=fc, sl=sl: self.dma('pool', wst[:, sl, :], self.w_out[l, fc * 128:(fc + 1) * 128, :], (), [Rwst[sl]], owner=Rwst[sl]))
            pcs.append(lambda fc=fc, sl=sl: self.stt(wob[:], wst[:, sl, :], gain[:, fc:fc + 1], gm[:], ALU.mult, ALU.mult,
                                                     [Rwst[sl], Rgm], [Rwob]))
            pcs.append(lambda fc=fc: self.dma('pool', self.WO[:, fc, :], wob[:], [Rwob], (), owner=Rwob))
        for e in range(16):
            for gi, wsrc in enumerate((self.w_gate, self.w_up)):
                pcs.append(lambda e=e, gi=gi, wsrc=wsrc: self.dma('pool', self.WGU[e][:, :, gi * 256:(gi + 1) * 256],
                                                                  wsrc[l, e].rearrange("(kc p) n -> p kc n", p=128), (), (), owner=Rcv))
            for nh in range(2):
                pcs.append(lambda e=e, nh=nh: self.dma('pool', self.WD[nh, e // 4, :, (e % 4) * 2:(e % 4) * 2 + 2, :],
                                                       self.w_down[l, e, :, nh * 512:(nh + 1) * 512].rearrange("(kc p) n -> p kc n", p=128),
                                                       (), (), owner=Rcv))
        return pcs

    def phase_A(self, l):
        nc, s = self.nc, self.s
        T, NCH = self.T, self.NCH
        xsrc = self.x_in if l == 0 else self.xs
        RC, RM = self.R_const, self.R_mod
        with ExitStack() as ph:
            def A(name, shape, dtype):
                return ph.enter_context(nc.sbuf_tensor(self.un(name), list(shape), dtype))

            def P(name, shape, dtype):
                return ph.enter_context(nc.psum_tensor(self.un(name), list(shape), dtype))
            winb = A("winb", [128, 8, IN_COLS], BF16); Rwin = s.res("winb")
            wstg = A("wstg", [128, 2, IN_COLS], F32); Rwstg = [s.res("wstg0"), s.res("wstg1")]
            for kc in range(8):
                sl = kc % 2
                self.dma('sp', wstg[:, sl, :], self.w_in[l, kc * 128:(kc + 1) * 128, :], (), [Rwstg[sl]], owner=Rwstg[sl])
                self.cp('pool', winb[:, kc, :], wstg[:, sl, :], [Rwstg[sl]], [Rwin])
            negbf = A("negbf", [4, 1], F32)
            Rp = s.res("lrup")
            self.dma('sp', negbf[:], self.fox_bf[l].rearrange("(h o) -> h o", o=1), (), [Rp], owner=Rp)
            self.ts('dve', negbf[:], negbf[:], -1.0, None, ALU.mult, None, [Rp], [Rp])

            xt = A("xt", [128, 4, D], F32); Rxt = [s.res(f"xt{i}") for i in range(4)]
            junk = A("junk", [128, D], BF16); Rjunk = s.res("junk")
            st = A("st", [128, 4, 4], F32); Rst = [s.res(f"st{i}") for i in range(4)]
            u = A("u", [128, 4, D], F32); Ru = [s.res(f"u{i}") for i in range(4)]
            hb = A("hb", [128, 4, D], BF16); Rhb = [s.res(f"hb{i}") for i in range(4)]
            hT = A("hT", [128, 2, 8, 512], BF16); RhT = [s.res("hT0"), s.res("hT1")]
            ptr = [P(f"ptr{i}", [128, 8, 128], BF16) for i in range(2)]; Rptr = [s.res("ptr0"), s.res("ptr1")]
            pp = [P(f"pp{i}", [128, 512], F32) for i in range(4)]; Rpp = [s.res(f"pp{i}") for i in range(4)]
            pq = [P(f"pq{i}", [128, 512], F32) for i in range(2)]; Rpq = [s.res(f"pq{i}") for i in range(2)]
            NST = 4
            stg = A("stg", [128, NST, 512], BF16); Rstg = [s.res(f"stg{i}") for i in range(NST)]
            vstg = A("vstg", [128, 2, 384], BF16); Rvstg = [s.res("vstg0"), s.res("vstg1")]
            fstg = A("fstg", [128, 4, 512], F32); Rfstg = [s.res(f"fstg{i}") for i in range(4)]
            ifs = 0
            fT = A("fT", [4, 512], F32); RfT = s.res("fT")
            spx = A("spx", [4, 512], F32); Rspx = s.res("spx")
            rr_ = A("rr", [4, 512], F32); Rrr = s.res("rr")
            cg = A("cg", [4, 512], F32); Rcg = s.res("cg")
            rhi = A("rhi", [4, 512], BF16); rlo = A("rlo", [4, 512], BF16); Rrh = s.res("rhl")
            rt = A("rt", [4, 512], F32); Rrt = s.res("rt")
            fcar = A("fcar", [4, 1], F32); Rfc = s.res("fcar")
            cbt = A("cbt", [4, 128], F32); Rcbt = s.res("cbt")
            self.memset('pool', fcar[:], 0.0, [Rfc])
            self.memset('pool', rt[:], 1.0, [Rrt])
            ist = 0
            ipp = 0
            conv = self.conv_pieces(l, A)
            if getattr(self, 'verbose', False):
                print("phase A sbuf remaining", nc.sbuf_bytes_remaining)
            nslots = NCH * 17
            cvi = 0
            cvslot = 0

            def conv_tick():
                nonlocal cvi, cvslot
                cvslot += 1
                tgt = (len(conv) * cvslot) // nslots
                while cvi < min(tgt, len(conv)):
                    conv[cvi]()
                    cvi += 1
            FM = [('qa', 0), ('qa', 128), ('qa', 256), ('qa', 384), ('ka', 512),
                  ('xb', 768), ('xb', 896), ('gb', 1024), ('gb', 1152),
                  ('qc', 1280), ('qc', 1408), ('kc', 1536), ('kc', 1664)]
            def prepA(c, i):
                tok = c * 512 + i * 128
                sl = i
                self.dma('sp', xt[:, sl, :], xsrc[tok:tok + 128, :], (), [Rxt[sl]], owner=Rxt[sl])
                self.act(junk[:], xt[:, sl, :], AF.Square, [Rxt[sl]], [Rjunk, Rst[sl]], accum_out=st[:, sl, 0:1])
                self.ts('dve', st[:, sl, 1:2], st[:, sl, 0:1], 1.0 / D, EPS, ALU.mult, ALU.add, [Rst[sl]], [Rst[sl]])
                self.act(st[:, sl, 2:3], st[:, sl, 1:2], AF.Sqrt, [Rst[sl]], [Rst[sl]])
                self.recip(st[:, sl, 3:4], st[:, sl, 2:3], [Rst[sl]], [Rst[sl]])
                self.stt(u[:, sl, :], xt[:, sl, :], st[:, sl, 3:4], self.A_mix[:], ALU.mult, ALU.mult,
                         [Rxt[sl], Rst[sl], RM], [Ru[sl]])
                self.tt('pool', hb[:, sl, :], u[:, sl, :], self.shm[:], ALU.add, [Ru[sl], RM], [Rhb[sl]])

            def prepB(c, i):
                sl = i
                pslt = (c * 4 + i) % 2
                for dc in range(8):
                    self.tr(ptr[pslt][:, dc, :], hb[:, sl, dc * 128:(dc + 1) * 128], self.ident_b[:], [Rhb[sl], RC], [Rptr[pslt]])
                self.cp('act', hT[:, c % 2, :, i * 128:(i + 1) * 128], ptr[pslt][:], [Rptr[pslt]], [RhT[c % 2]])
            for i in range(4):
                prepA(0, i)
            for i in range(4):
                prepB(0, i)
            pk = 0
            cur_c = 0
            real_tick = conv_tick

            def conv_tick():
                nonlocal pk
                real_tick()
                pk += 1
                cn = cur_c + 1
                if cn < NCH:
                    if pk % 4 == 1:
                        i_ = pk // 4
                        if i_ < 4:
                            prepA(cn, i_)
                    if pk % 4 == 3:
                        i_ = pk // 4
                        if i_ < 4:
                            prepB(cn, i_)
            for c in range(NCH):
                hs_ = c % 2
                cols = slice(c * 512, (c + 1) * 512)
                pk = 0
                cur_c = c
                for i in range(4):
                    tok = c * 512 + i * 128
                    b = ipp % 4; ipp += 1
                    vs = i % 2
                    for dc in range(8):
                        self.mm(pp[b][:, 0:128], hT[:, hs_, dc, i * 128:(i + 1) * 128], winb[:, dc, 640:768], dc == 0, dc == 7,
                                [RhT[hs_], Rwin], [Rpp[b]])
                    for dc in range(8):
                        self.mm(pp[b][:, 128:384], hT[:, hs_, dc, i * 128:(i + 1) * 128], winb[:, dc, 1792:2048], dc == 0, dc == 7,
                                [RhT[hs_], Rwin], [Rpp[b]])
                    self.evac(vstg[:, vs, :], pp[b][:, 0:384], [Rpp[b]], [Rvstg[vs]])
                    self.dma('pool', self.Va[tok:tok + 128, :], vstg[:, vs, 0:128], [Rvstg[vs]], (), owner=Rvstg[vs])
                    self.dma('pool', self.Vc[tok:tok + 128, :], vstg[:, vs, 128:384], [Rvstg[vs]], (), owner=Rvstg[vs])
                    conv_tick()
                for (nm, col) in FM:
                    b = ipp % 4; ipp += 1
                    for dc in range(8):
                        self.mm(pp[b][:], winb[:, dc, col:col + 128], hT[:, hs_, dc, :], dc == 0, dc == 7,
                                [Rwin, RhT[hs_]], [Rpp[b]])
                    if nm in ('qa', 'ka', 'qc', 'kc'):
                        si = ist % NST; ist += 1
                        self.evac(stg[:, si, :], pp[b][:], [Rpp[b]], [Rstg[si]], scale=(0.125 if nm[0] == 'q' else None))
                        if nm == 'qa':
                            self.dma('pool', self.QaT[col:col + 128, cols], stg[:, si, :], [Rstg[si]], (), owner=Rstg[si])
                        elif nm == 'ka':
                            self.dma('pool', self.KaT[:, cols], stg[:, si, :], [Rstg[si]], (), owner=Rstg[si])
                        else:
                            dst = self.QcT if nm == 'qc' else self.KcT
                            h0 = (col - (1280 if nm == 'qc' else 1536)) // 64
                            self.dma('pool', dst[h0, 0:64, cols], stg[0:64, si, :], [Rstg[si]], (), owner=Rstg[si])
                            self.dma('pool', dst[h0 + 1, 0:64, cols], stg[64:128, si, :], [Rstg[si]], (), owner=Rstg[si])
                    else:
                        k = (col - (768 if nm == 'xb' else 1024)) // 128
                        fi = ifs % 4; ifs += 1
                        self.evac(fstg[:, fi, :], pp[b][:], [Rpp[b]], [Rfstg[fi]])
                        dst = self.XB if nm == 'xb' else self.GB
                        self.dma('pool', dst[k * 128:(k + 1) * 128, cols], fstg[:, fi, :], [Rfstg[fi]], (), owner=Rfstg[fi])
                        if nm == 'xb' and c == NCH - 1:
                            o_ = self.NT * 4 + 4 + k * 3
                            self.cp('dve', self.exc[:, o_:o_ + 3], fstg[:, fi, 509:512], [Rfstg[fi]], [self.R_cum])
                    conv_tick()
                b = ipp % 4; ipp += 1
                for dc in range(8):
                    self.mm(pp[b][0:4, :], winb[:, dc, 2048:2052], hT[:, hs_, dc, :], dc == 0, dc == 7, [Rwin, RhT[hs_]], [Rpp[b]])
                self.evac(fT[:], pp[b][0:4, :], [Rpp[b]], [RfT], eng='dve')
                self.act(spx[:], fT[:], AF.Exp, [RfT, Rp], [Rspx], bias=negbf[:], scale=-1.0)
                self.act(spx[:], spx[:], AF.Ln, [Rspx], [Rspx], bias=1.0)
                self.scan(rr_[:], rt[:], spx[:], 0.0, ALU.mult, ALU.subtract, [Rspx, Rrt], [Rrr])
                self.cp('dve', cbt[:], fcar[:].to_broadcast([4, 128]), [Rfc], [Rcbt])
                self.mm(pq[0][:, 0:4], cbt[:], self.ident_f[0:4, 0:4], True, True, [Rcbt, RC], [Rpq[0]])
                self.cp('dve', self.Cbc[:, c, :], pq[0][:, 0:4], [Rpq[0]], [self.R_cum])
                self.ts('dve', cg[:], rr_[:], fcar[:], None, ALU.add, None, [Rrr, Rfc], [Rcg])
                self.cp('dve', fcar[:], cg[:, 511:512], [Rcg], [Rfc])
                for i in range(4):
                    self.tr(pq[1][:, i * 4:(i + 1) * 4], cg[:, i * 128:(i + 1) * 128], self.ident_f[0:4, 0:4], [Rcg, RC], [Rpq[1]])
                self.cp('dve', self.cumK[:, c * 4:(c + 1) * 4, :], pq[1][:, 0:16].rearrange("p (a b) -> p a b", a=4),
                        [Rpq[1]], [self.R_cum])
                self.cp('dve', rhi[:], rr_[:], [Rrr], [Rrh])
                self.tt('dve', rlo[:], rr_[:], rhi[:], ALU.subtract, [Rrr, Rrh], [Rrh])
                self.dma('pool', self.QcT[:, 64, cols], rhi[:], [Rrh], (), owner=Rrh)
                self.dma('pool', self.QcT[:, 65, cols], rlo[:], [Rrh], (), owner=Rrh)
            while cvi < len(conv):
                conv[cvi]()
                cvi += 1
            self.cp('dve', cbt[:], fcar[:].to_broadcast([4, 128]), [Rfc], [Rcbt])
            self.mm(pq[0][:, 0:4], cbt[:], self.ident_f[0:4, 0:4], True, True, [Rcbt, RC], [Rpq[0]])
            self.cp('dve', self.exc[:, self.NT * 4:self.NT * 4 + 4], pq[0][:, 0:4], [Rpq[0]], [self.R_cum])
            Rex = s.res("exc")
            self.dma('sp', self.EXC, self.exc[:], [self.R_cum], (), owner=Rex)
            if self.dbg:
                self.dma('sp', self.dbgcum, self.cumK, [self.R_cum], (), owner=Rrt)
            s.barrier()
            self.dma('sp', self.EXS[:, 0:128], self.KaT[:, T - 128:T], (), (), owner=Rex)
            self.dma('sp', self.EXS[:, 128:256], self.Va[T - 128:T, :], (), (), owner=Rex)
            for hp in range(2):
                self.allgather(self.KcT[2 * hp:2 * hp + 2].rearrange("h r t -> (h r) t"), self.KcTg[hp], "cc_k")
            self.allgather(self.Vc, self.Vcg, "cc_v")
            self.allgather(self.EXC, self.EXCg, "cc_c")
            self.allgather(self.EXS, self.EXSg, "cc_s")
            s.barrier()
            s.flush()

    def lru_pieces(self, l, A, px, Rpx):
        s = self.s
        T, NCH, NT = self.T, self.NCH, self.NT
        RC = self.R_const
        pcs = []
        cw = A("cw", [128, 2, 4], F32); cb = A("cb", [128, 2], F32)
        ba = A("ba", [128, 2], F32); bx = A("bx", [128, 2], F32); lam = A("lam", [128, 2], F32)
        cL = A("cL", [128, 2], F32); cL2 = A("cL2", [128, 2], F32); tmpl = A("tmpl", [128, 2], F32)
        wabd = A("wabd", [128, 2, 128], F32); wxbd = A("wxbd", [128, 2, 128], F32)
        Rp = s.res("lrup")
        xbT = A("xbT", [128, 2, 2, 515], F32); Rxb = [s.res("xb0"), s.res("xb1")]
        gbT = A("gbT", [128, 2, 2, 512], F32); Rgb = [s.res("gb0"), s.res("gb1")]
        xc = A("xc", [128, 512], F32); Rxc = s.res("xc")
        rg = A("rg", [128, 512], F32); Rrg = s.res("rg")
        ig = A("ig", [128, 512], F32); Rig = s.res("ig")
        aa = A("aa", [128, 512], F32); Raa = s.res("aa")
        sq = A("sq", [128, 512], F32); Rsq = s.res("sq")
        uu = A("uu", [128, 512], F32); Ruu = s.res("uu")
        hs = A("hs", [128, 512], F32); Rhs = s.res("hs")
        ac = A("ac", [128, 512], F32); Rac = s.res("ac")
        zz = A("zz", [128, 512], F32); Rzz = s.res("zz")
        gg = A("gg", [128, 512], F32); Rgg = s.res("gg")
        g2 = A("g2", [128, 512], F32); Rg2 = s.res("g2")
        hcar = A("hcar", [128, 2], F32); Rhc = s.res("hcar")
        acar = A("acar", [128, 2], F32); Rac2 = s.res("acar")
        exh = A("exh", [128, 16], F32); Rexh = s.res("exh")
        self.lru_exh = (exh, Rexh)
        lstg = A("lstg", [128, 2, 512], BF16); Rstg = [s.res("lstg0"), s.res("lstg1")]
        ctail = A("ctail", [128, 2, 3], F32); Rct = s.res("ctail")

        def init():
            self.memset('pool', wabd[:], 0.0, [Rp])
            self.memset('pool', wxbd[:], 0.0, [Rp])
            for (dst, src) in ((cw, self.convT), (cb, self.convb), (ba, self.lru_ba), (bx, self.lru_bx), (lam, self.lru_lam)):
                self.dma('pool', dst[:], src[l], (), [Rp], owner=Rp)
            for b in range(8):
                k, q = b // 4, (b % 4) * 32
                self.dma('pool', wabd[q:q + 32, k, q:q + 32], self.lru_wa[l, b], (), [Rp], owner=Rp)
                self.dma('pool', wxbd[q:q + 32, k, q:q + 32], self.lru_wx[l, b], (), [Rp], owner=Rp)
            self.memset('pool', zz[:], 0.0, [Rzz])
            self.memset('pool', hcar[:], 0.0, [Rhc])
            self.memset('pool', acar[:], 1.0, [Rac2])
            self.memset('pool', exh[:], 0.0, [Rexh])
            o_ = self.NT * 4 + 4
            self.dma('pool', ctail[:], self.EXCg[0:128, o_:o_ + 6].rearrange("p (a b) -> p a b", a=2), (), [Rct], owner=Rct)
        pcs.append(init)

        def init2():
            self.act(tmpl[:], lam[:], AF.Exp, [Rp], [Rp], scale=-1.0)
            self.act(tmpl[:], tmpl[:], AF.Ln, [Rp], [Rp], bias=1.0)
            self.ts('dve', xbT[:, 0, :, 0:3], ctail[:], self.flg[:, 0:1], None, ALU.mult, None, [Rct, RC], [Rxb[0]])
        pcs.append(init2)

        def init3():
            self.ts('dve', cL[:], tmpl[:], -8.0, None, ALU.mult, None, [Rp], [Rp])
            self.ts('dve', cL2[:], tmpl[:], -16.0, None, ALU.mult, None, [Rp], [Rp])
        pcs.append(init3)
        for c in range(NCH):
            sl = c % 2
            cols = slice(c * 512, (c + 1) * 512)

            def ld(c=c):
                sl_ = c % 2
                cols_ = slice(c * 512, (c + 1) * 512)
                if c > 0:
                    self.cp('pool', xbT[:, sl_, :, 0:3], xbT[:, 1 - sl_, :, 512:515], [Rxb[1 - sl_]], [Rxb[sl_]])
                self.dma('sp', xbT[:, sl_, :, 3:515], self.XB[:, cols_].rearrange("(k p) t -> p k t", p=128), (), [Rxb[sl_]], owner=Rxb[sl_])
                self.dma('sp', gbT[:, sl_], self.GB[:, cols_].rearrange("(k p) t -> p k t", p=128), (), [Rgb[sl_]], owner=Rgb[sl_])
            if c == 0:
                pcs.append(ld)
            if c + 1 < NCH:
                pcs.append(lambda c=c: ld(c + 1))
            for k in range(2):
                def s1(sl=sl, k=k):
                    self.ts('dve', xc[:], xbT[:, sl, k, 0:512], cw[:, k, 0:1], cb[:, k:k + 1], ALU.mult, ALU.add, [Rxb[sl], Rp], [Rxc])
                    for j in range(1, 4):
                        self.stt(xc[:], xbT[:, sl, k, j:j + 512], cw[:, k, j:j + 1], xc[:], ALU.mult, ALU.add, [Rxb[sl], Rp, Rxc], [Rxc])
                    self.act(g2[:], gbT[:, sl, k, :], AF.Square, [Rgb[sl]], [Rg2])

                def s2(sl=sl, k=k):
                    self.mm(px[:], wabd[:, k, :], xc[:], True, True, [Rp, Rxc], [Rpx])
                    self.ts('dve', g2[:], g2[:], 0.044715, 1.0, ALU.mult, ALU.add, [Rg2], [Rg2])

                def s3(sl=sl, k=k):
                    self.act(rg[:], px[:], AF.Sigmoid, [Rpx, Rp], [Rrg], bias=ba[:, k:k + 1])
                    self.tt('pool', g2[:], g2[:], gbT[:, sl, k, :], ALU.mult, [Rg2, Rgb[sl]], [Rg2])

                def s4(sl=sl, k=k):
                    self.mm(px[:], wxbd[:, k, :], xc[:], True, True, [Rp, Rxc], [Rpx])
                    self.act(aa[:], rg[:], AF.Exp, [Rrg, Rp], [Raa], scale=cL[:, k:k + 1])
                    self.act(sq[:], rg[:], AF.Exp, [Rrg, Rp], [Rsq], scale=cL2[:, k:k + 1])

                def s5(sl=sl, k=k):
                    self.act(ig[:], px[:], AF.Sigmoid, [Rpx, Rp], [Rig], bias=bx[:, k:k + 1])
                    self.act(sq[:], sq[:], AF.Sqrt, [Rsq], [Rsq], bias=1.0, scale=-1.0)
                    self.act(gg[:], g2[:], AF.Sigmoid, [Rg2], [Rgg], scale=1.5957691216057308)
                    self.scan(ac[:], aa[:], zz[:], acar[:, k:k + 1], ALU.mult, ALU.add, [Raa, Rzz, Rac2], [Rac])
                    self.cp('dve', acar[:, k:k + 1], ac[:, 511:512], [Rac], [Rac2])

                def s6(sl=sl, k=k):
                    self.tt('pool', uu[:], ig[:], xc[:], ALU.mult, [Rig, Rxc], [Ruu])
                    self.tt('pool', uu[:], uu[:], sq[:], ALU.mult, [Ruu, Rsq], [Ruu])
                    self.tt('pool', gg[:], gg[:], gbT[:, sl, k, :], ALU.mult, [Rgg, Rgb[sl]], [Rgg])

                def s7(sl=sl, k=k):
                    self.scan(hs[:], aa[:], uu[:], hcar[:, k:k + 1], ALU.mult, ALU.add, [Raa, Ruu, Rhc], [Rhs])
                    self.cp('dve', hcar[:, k:k + 1], hs[:, 511:512], [Rhs], [Rhc])
                    self.tt('pool', lstg[:, 1, :], ac[:], gg[:], ALU.mult, [Rac, Rgg], [Rstg[1]])

                def s8(sl=sl, k=k, cols=cols):
                    self.tt('dve', lstg[:, 0, :], hs[:], gg[:], ALU.mult, [Rhs, Rgg], [Rstg[0]])
                    self.dma('pool', self.ZT[k * 128:(k + 1) * 128, cols], lstg[:, 1, :], [Rstg[1]], (), owner=Rstg[1])

                def s9(sl=sl, k=k, cols=cols):
                    self.dma('pool', self.YT[512 + k * 128:512 + (k + 1) * 128, cols], lstg[:, 0, :], [Rstg[0]], (), owner=Rstg[0])
                pcs.extend([s1, s2, s3, s4, s5, s6, s7, s8, s9])

        def fin():
            self.cp('dve', exh[:, 0:2], hcar[:], [Rhc], [Rexh])
        pcs.append(fin)
        return pcs

    def _normalize(self, po, Rpo, pb, Rpb, rd, Rrd, osb, Rosb, out_ap, Rout, sink_ap=None):
        if sink_ap is not None:
            self.tt('dve', rd[64:65, :], po[64:65, :], sink_ap, ALU.add, [Rpo, self.R_const], [Rrd])
            self.recip(rd[64:65, :], rd[64:65, :], [Rrd], [Rrd])
        else:
            self.recip(rd[64:65, :], po[64:65, :], [Rpo], [Rrd])
        self.mm(pb[0:64, :], self.ones_f[64:65, 0:64], rd[64:65, :], True, True, [self.R_const, Rrd], [Rpb])
        self.cp('act', osb[0:64, :], po[0:64, :], [Rpo], [Rosb])
        i0, i1 = osb[0:64, :], pb[0:64, :]
        if len(out_ap.shape) == 3:
            i0 = i0.rearrange("p (a b) -> p a b", a=4)
            i1 = i1.rearrange("p (a b) -> p a b", a=4)
        self.tt('dve', out_ap, i0, i1, ALU.mult, [Rosb, Rpb], [Rout])

    def phase_swa(self, l):
        nc, s = self.nc, self.s
        T, NCH, NT = self.T, self.NCH, self.NT
        RC = self.R_const
        with ExitStack() as ph:
            def A(name, shape, dtype):
                return ph.enter_context(nc.sbuf_tensor(self.un(name), list(shape), dtype))

            def P(name, shape, dtype):
                return ph.enter_context(nc.psum_tensor(self.un(name), list(shape), dtype))
            amask = A("amask", [128, 2, 2, 4, 128], BF16); Rm = s.res("amask")
            dd = A("dd", [128, 128], F32); d2 = A("d2", [128, 128], F32); Rt = s.res("tmp")
            s.op('pool', lambda e: e.iota(dd[:], [[1, 128]], base=0, channel_multiplier=-1,
                                          allow_small_or_imprecise_dtypes=True), (), [Rt])
            for hq in range(8):
                g, hh = hq // 4, hq % 4
                slope = 2.0 ** (-(hq + 1))
                self.ts('pool', d2[:], dd[:], -slope, None, ALU.mult, None, [Rt], [Rt])
                self.asel(amask[:, g, 1, hh, :], d2[:], [[1, 128]], ALU.is_ge, NEG, 0, -1, [Rt], [Rm])
                self.ts('pool', d2[:], dd[:], 128.0, -slope, ALU.add, ALU.mult, [Rt], [Rt])
                self.asel(amask[:, g, 0, hh, :], d2[:], [[-1, 128]], ALU.is_ge, NEG, -1, 1, [Rt], [Rm])
            sk = A("sk", [64, 8], F32); sinkb = A("sinkb", [64, 2, 4, 128], F32); Rsk = s.res("sk")
            self.dma('sp', sk[:], self.sinks[l].partition_broadcast(64), (), [Rsk], owner=Rsk)
            self.act(sk[:], sk[:], AF.Exp, [Rsk], [Rsk])
            for g in range(2):
                self.cp('dve', sinkb[:, g], sk[:, g * 4:(g + 1) * 4].unsqueeze(2).to_broadcast([64, 4, 128]), [Rsk], [RC])
            kat = A("kat", [64, 2, 128 + T], BF16); Rkat = [s.res("kat0"), s.res("kat1")]
            vaa = A("vaa", [128, 2, NT + 1, 65], BF16); Rvaa = s.res("vaa")
            qat = A("qat", [64, 2, 4, 512], BF16); Rqat = [s.res("qat0"), s.res("qat1")]
            ya = A("ya", [64, 2, 4, 512], BF16); Rya = [s.res("ya0"), s.res("ya1")]
            pt = A("pt", [128, 4, 512], BF16); Rpt = [s.res(f"pt{i}") for i in range(4)]
            rd = A("rd", [128, 2, 512], F32); Rrd = [s.res("rd0"), s.res("rd1")]
            osb = A("osb", [64, 2, 512], F32); Rosb = [s.res("osb0"), s.res("osb1")]
            ps = [P(f"ps{i}", [128, 512], F32) for i in range(4)]; Rps = [s.res(f"ps{i}") for i in range(4)]
            po = [P(f"po{i}", [128, 512], F32) for i in range(2)]; Rpo = [s.res(f"po{i}") for i in range(2)]
            pb = [P(f"pb{i}", [128, 512], F32) for i in range(2)]; Rpb = [s.res(f"pb{i}") for i in range(2)]
            self.memset('pool', vaa[:], 1.0, [Rvaa])
            units = [(g, c, i) for g in range(2) for c in range(NCH) for i in range(4)]

            def stage1(u):
                g, c, i = units[u]
                sl = c % 2
                if c == 0 and i == 0:
                    gs = g % 2
                    self.dma('sp', kat[:, gs, 128:], self.KaT[g * 64:(g + 1) * 64, :], (), [Rkat[gs]], owner=Rkat[gs])
                    self.dma('sp', kat[:, gs, 0:128], self.EXSg[g * 64:(g + 1) * 64, 0:128], (), [Rkat[gs]], owner=Rkat[gs])
                    self.dma('sp', vaa[:, gs, 1:, 0:64], self.Va[:, g * 64:(g + 1) * 64].rearrange("(j p) d -> p j d", p=128),
                             (), [Rvaa], owner=Rvaa)
                    self.dma('sp', vaa[:, gs, 0, 0:64], self.EXSg[0:128, 128 + g * 64:128 + (g + 1) * 64], (), [Rvaa], owner=Rvaa)
                if i == 0:
                    cols = slice(c * 512, (c + 1) * 512)
                    self.dma('sp', qat[:, sl], self.QaT[g * 256:(g + 1) * 256, cols].rearrange("(h d) t -> d h t", d=64),
                             (), [Rqat[sl]], owner=Rqat[sl])
                n = c * 4 + i
                for kb in range(2):
                    blk = n + kb
                    b = (u % 2) * 2 + kb
                    self.mm(ps[b][:].rearrange("p (a b) -> p a b", a=4), kat[:, g % 2, blk * 128:(blk + 1) * 128],
                            qat[:, sl, :, i * 128:(i + 1) * 128], True, False, [Rkat[g % 2], Rqat[sl]], [Rps[b]])
                    self.mm(ps[b][:].rearrange("p (a b) -> p a b", a=4), self.ident_b[:], amask[:, g, kb], False, True,
                            [RC, Rm], [Rps[b]])

            def stage2(u):
                g, c, i = units[u]
                n = c * 4 + i
                o = u % 2
                for kb in range(2):
                    blk = n + kb
                    b = (u % 2) * 2 + kb
                    if blk == 0:
                        self.act(pt[:, b, :], ps[b][:], AF.Exp, [Rps[b], RC], [Rpt[b]], bias=self.flg[:, 1:2])
                    else:
                        self.act(pt[:, b, :], ps[b][:], AF.Exp, [Rps[b]], [Rpt[b]])
                for kb in range(2):
                    blk = n + kb
                    b = (u % 2) * 2 + kb
                    self.mm(po[o][0:65, :], vaa[:, g % 2, blk, 0:65], pt[:, b, :], kb == 0, kb == 1, [Rvaa, Rpt[b]], [Rpo[o]])

            def stage3(u):
                g, c, i = units[u]
                sl = c % 2
                o = u % 2
                rdo = rd[0:64, o, :]
                self.tt('dve', rdo, pb[o][0:64, :], sinkb[:, g].rearrange("p a b -> p (a b)"), ALU.add, [Rpb[o], RC], [Rrd[o]])
                self.recip(rdo, rdo, [Rrd[o]], [Rrd[o]])
                self.cp('act', osb[0:64, o, :], po[o][0:64, :], [Rpo[o]], [Rosb[o]])
                self.tt('dve', ya[:, sl, :, i * 128:(i + 1) * 128], osb[0:64, o, :].rearrange("p (a b) -> p a b", a=4),
                        rdo.rearrange("p (a b) -> p a b", a=4), ALU.mult, [Rosb[o], Rrd[o]], [Rya[sl]])
                if i == 3:
                    cols = slice(c * 512, (c + 1) * 512)
                    self.dma('pool', self.YT[g * 256:(g + 1) * 256, cols].rearrange("(h d) t -> d h t", d=64), ya[:, sl],
                             [Rya[sl]], (), owner=Rya[sl])
            NU = len(units)
            for q in range(NU + 2):
                if q < NU:
                    stage1(q)
                if 0 <= q - 1 < NU:
                    stage2(q - 1)
                if 0 <= q - 2 < NU:
                    stage3(q - 2)
            s.barrier()
            s.flush()

    def phase_fox(self, l):
        nc, s = self.nc, self.s
        T, NCH, NT = self.T, self.NCH, self.NT
        RC = self.R_const
        with ExitStack() as ph:
            def A(name, shape, dtype):
                return ph.enter_context(nc.sbuf_tensor(self.un(name), list(shape), dtype))

            def P(name, shape, dtype):
                return ph.enter_context(nc.psum_tensor(self.un(name), list(shape), dtype))
            fmask = A("fmask", [128, 4, 512], BF16); Rm = s.res("fmask")
            zt = A("zt", [128, 512], F32); Rt = s.res("tmp")
            self.memset('pool', zt[:], 0.0, [Rt])
            for m in range(4):
                self.asel(fmask[:, m, :], zt[:], [[1, 512]], ALU.is_ge, NEG, -128 * m, -1, [Rt], [Rm])
            kt = A("kt", [66, 2, 2 * T], BF16); Rkt = [s.res("kt0"), s.res("kt1")]
            vca = A("vca", [128, 2, 2 * NT, 65], BF16); Rvca = [s.res("vca0"), s.res("vca1")]
            cumP = A("cumP", [128, NT * 4 + 16], F32); Rcp = s.res("cumP")
            self.dma('sp', cumP[:], self.EXCg[0:128, :], (), [Rcp], owner=Rcp)
            cpv = cumP[:, 0:NT * 4].rearrange("p (a b) -> p a b", b=4)
            poff = A("poff", [128, 4], F32)
            self.ts('dve', poff[:], cumP[:, NT * 4:NT * 4 + 4], self.flg[:, 1:2], None, ALU.add, None, [Rcp, RC], [Rcp])
            cq = A("cq", [128, 2, 1], F32); Rcq = [s.res("cq0"), s.res("cq1")]
            qa = A("qa", [66, 2, 512], BF16); Rqa = [s.res("qa0"), s.res("qa1")]
            bt = A("bt", [128, 2, 2 * NT], F32); Rbt = [s.res("bt0"), s.res("bt1")]
            yc = A("yc", [64, 2, 512], BF16); Ryc = [s.res("yc0"), s.res("yc1")]
            pt = A("pt", [128, 3, 512], BF16); Rpt = [s.res(f"pt{i}") for i in range(3)]
            rd = A("rd", [128, 2, 512], F32); Rrd = [s.res("rd0"), s.res("rd1")]
            osb = A("osb", [64, 2, 512], F32); Rosb = [s.res("osb0"), s.res("osb1")]
            ps = [P(f"ps{i}", [128, 512], F32) for i in range(3)]; Rps = [s.res(f"ps{i}") for i in range(3)]
            po = [P(f"po{i}", [128, 512], F32) for i in range(2)]; Rpo = [s.res(f"po{i}") for i in range(2)]
            pb = [P(f"pb{i}", [128, 512], F32) for i in range(2)]; Rpb = [s.res(f"pb{i}") for i in range(2)]
            px = P("px", [128, 512], F32); Rpx = s.res("px")
            self.memset('pool', vca[:], 1.0, [Rvca[0], Rvca[1]])
            amask = A("amask", [128, 2, 2, 4, 128], BF16); Rma = s.res("amask")
            dd = A("dd", [128, 128], F32); d2 = A("d2", [128, 128], F32); Rtt = s.res("tmp2")
            s.op('pool', lambda e: e.iota(dd[:], [[1, 128]], base=0, channel_multiplier=-1,
                                          allow_small_or_imprecise_dtypes=True), (), [Rtt])
            for hq in range(8):
                g, hh = hq // 4, hq % 4
                slope = 2.0 ** (-(hq + 1))
                self.ts('pool', d2[:], dd[:], -slope, None, ALU.mult, None, [Rtt], [Rtt])
                self.asel(amask[:, g, 1, hh, :], d2[:], [[1, 128]], ALU.is_ge, NEG, 0, -1, [Rtt], [Rma])
                self.ts('pool', d2[:], dd[:], 128.0, -slope, ALU.add, ALU.mult, [Rtt], [Rtt])
                self.asel(amask[:, g, 0, hh, :], d2[:], [[-1, 128]], ALU.is_ge, NEG, -1, 1, [Rtt], [Rma])
            sk = A("sk", [64, 8], F32); sinkb = A("sinkb", [64, 2, 4, 128], F32); Rsk = s.res("sk")
            self.dma('sp', sk[:], self.sinks[l].partition_broadcast(64), (), [Rsk], owner=Rsk)
            self.act(sk[:], sk[:], AF.Exp, [Rsk], [Rsk])
            for g in range(2):
                self.cp('dve', sinkb[:, g], sk[:, g * 4:(g + 1) * 4].unsqueeze(2).to_broadcast([64, 4, 128]), [Rsk], [RC])
            kat = A("kat", [64, 2, 128 + T], BF16); Rkat = [s.res("kat0"), s.res("kat1")]
            vaa = A("vaa", [128, 2, NT + 1, 65], BF16); Rvaa = [s.res("vaa0"), s.res("vaa1")]
            qat = A("qat", [64, 2, 4, 512], BF16); Rqat = [s.res("qat0"), s.res("qat1")]
            ya = A("ya", [64, 2, 4, 512], BF16); Rya = [s.res("ya0"), s.res("ya1")]
            self.memset('pool', vaa[:], 1.0, [Rvaa[0], Rvaa[1]])
            main_side = self.lru_pieces(l, A, px, Rpx)
            if l + 1 < self.L:
                main_side = main_side + self.mod_ahead_pieces(l + 1, A, px, Rpx)
            conv_side = []
            side = []
            i1 = i2 = 0
            while i1 < len(main_side) or i2 < len(conv_side):
                if i1 < len(main_side):
                    side.append(main_side[i1]); i1 += 1
                if i2 < len(conv_side):
                    side.append(conv_side[i2]); i2 += 1
            jobs = []
            for g in range(2):
                for c in range(NCH):
                    for i in range(4):
                        for kb in range(2):
                            jobs.append((-1 - g, c, i * 2 + kb, 2))
            for h in range(4):
                for c in range(NCH):
                    nj = NT + 4 * c + 4
                    for j in range(nj):
                        jobs.append((h, c, j, nj))
            LA = 2
            chunk_idx = {}
            for q, (h, c, j, nj) in enumerate(jobs):
                key = (h, c, j // 2) if h < 0 else (h, c)
                if key not in chunk_idx:
                    chunk_idx[key] = len(chunk_idx)

            def swa1(q):
                h, c, j, _ = jobs[q]
                g = -1 - h
                i, kb = j // 2, j % 2
                sl = (g * NCH + c) % 2
                if c == 0 and j == 0:
                    self.dma('sp', kat[:, g, 128:], self.KaT[g * 64:(g + 1) * 64, :], (), [Rkat[g]], owner=Rkat[g])
                    self.dma('sp', kat[:, g, 0:128], self.EXSg[g * 64:(g + 1) * 64, 0:128], (), [Rkat[g]], owner=Rkat[g])
                    self.dma('sp', vaa[:, g, 1:, 0:64], self.Va[:, g * 64:(g + 1) * 64].rearrange("(j p) d -> p j d", p=128),
                             (), [Rvaa[g]], owner=Rvaa[g])
                    self.dma('sp', vaa[:, g, 0, 0:64], self.EXSg[0:128, 128 + g * 64:128 + (g + 1) * 64], (), [Rvaa[g]], owner=Rvaa[g])
                if j == 0:
                    cols = slice(c * 512, (c + 1) * 512)
                    self.dma('sp', qat[:, sl], self.QaT[g * 256:(g + 1) * 256, cols].rearrange("(h d) t -> d h t", d=64),
                             (), [Rqat[sl]], owner=Rqat[sl])
                blk = c * 4 + i + kb
                b = q % 3
                self.mm(ps[b][:].rearrange("p (a b) -> p a b", a=4), kat[:, g, blk * 128:(blk + 1) * 128],
                        qat[:, sl, :, i * 128:(i + 1) * 128], True, False, [Rkat[g], Rqat[sl]], [Rps[b]])
                self.mm(ps[b][:].rearrange("p (a b) -> p a b", a=4), self.ident_b[:], amask[:, g, kb], False, True,
                        [RC, Rma], [Rps[b]])

            def swa2(q):
                h, c, j, _ = jobs[q]
                g = -1 - h
                i, kb = j // 2, j % 2
                blk = c * 4 + i + kb
                b = q % 3
                o = chunk_idx[(h, c, i)] % 2
                if blk == 0:
                    self.act(pt[:, b, :], ps[b][:], AF.Exp, [Rps[b], RC], [Rpt[b]], bias=self.flg[:, 1:2])
                else:
                    self.act(pt[:, b, :], ps[b][:], AF.Exp, [Rps[b]], [Rpt[b]])
                self.mm(po[o][0:64, :], vaa[:, g, blk, 0:64], pt[:, b, :], kb == 0, kb == 1, [Rvaa[g], Rpt[b]], [Rpo[o]])
                self.mm(pb[o][0:64, :], self.ones_b[:, 0:64], pt[:, b, :], kb == 0, kb == 1, [RC, Rpt[b]], [Rpb[o]])

            def swa3(q):
                h, c, j, _ = jobs[q]
                if j % 2 != 1:
                    return
                g = -1 - h
                i = j // 2
                sl = (g * NCH + c) % 2
                o = chunk_idx[(h, c, i)] % 2
                rdo = rd[0:64, o, :]
                self.tt('dve', rdo, pb[o][0:64, :], sinkb[:, g].rearrange("p a b -> p (a b)"), ALU.add, [Rpb[o], RC], [Rrd[o]])
                self.recip(rdo, rdo, [Rrd[o]], [Rrd[o]])
                self.cp('act', osb[0:64, o, :], po[o][0:64, :], [Rpo[o]], [Rosb[o]])
                self.tt('dve', ya[:, sl, :, i * 128:(i + 1) * 128], osb[0:64, o, :].rearrange("p (a b) -> p a b", a=4),
                        rdo.rearrange("p (a b) -> p a b", a=4), ALU.mult, [Rosb[o], Rrd[o]], [Rya[sl]])
                if i == 3:
                    cols = slice(c * 512, (c + 1) * 512)
                    self.dma('pool', self.YT[g * 256:(g + 1) * 256, cols].rearrange("(h d) t -> d h t", d=64), ya[:, sl],
                             [Rya[sl]], (), owner=Rya[sl])

            def stage1(q):
                h, c, j, nj = jobs[q]
                if h < 0:
                    return swa1(q)
                hs = h % 2
                ci = chunk_idx[(h, c)]
                sl = ci % 2
                if c == 0 and j == 0:
                    self.dma('sp', kt[:, hs, T:], self.KcT[h], (), [Rkt[hs]], owner=Rkt[hs])
                    self.dma('sp', kt[:, hs, 0:T], self.KcTg[h // 2, (h % 2) * 66:(h % 2) * 66 + 66, :], (), [Rkt[hs]], owner=Rkt[hs])
                    self.dma('sp', vca[:, hs, NT:, 0:64], self.Vc[:, h * 64:(h + 1) * 64].rearrange("(j p) d -> p j d", p=128),
                             (), [Rvca[hs]], owner=Rvca[hs])
                    self.dma('sp', vca[:, hs, 0:NT, 0:64], self.Vcg[0:T, h * 64:(h + 1) * 64].rearrange("(j p) d -> p j d", p=128),
                             (), [Rvca[hs]], owner=Rvca[hs])
                if j == 0:
                    cols = slice(c * 512, (c + 1) * 512)
                    self.dma('sp', qa[:, sl, :], self.QcT[h, :, cols], (), [Rqa[sl]], owner=Rqa[sl])
                    self.ts('dve', bt[:, sl, NT:nj], self.cumK[:, 0:nj - NT, h], -1.0, self.Cbc[:, c, h:h + 1], ALU.mult, ALU.add,
                            [self.R_cum], [Rbt[sl]])
                    self.tt('dve', cq[:, sl, :], self.Cbc[:, c, h:h + 1], poff[:, h:h + 1], ALU.add, [self.R_cum, Rcp], [Rcq[sl]])
                    self.ts('dve', bt[:, sl, 0:NT], cpv[:, :, h], -1.0, cq[:, sl, :], ALU.mult, ALU.add,
                            [Rcp, Rcq[sl]], [Rbt[sl]])
                b = q % 3
                diag = j >= NT + 4 * c
                self.mm(ps[b][:], kt[:, hs, j * 128:(j + 1) * 128], qa[:, sl, :], True, not diag, [Rkt[hs], Rqa[sl]], [Rps[b]])
                if diag:
                    self.mm(ps[b][:], self.ident_b[:], fmask[:, j - NT - 4 * c, :], False, True, [RC, Rm], [Rps[b]])

            def stage2(q):
                h, c, j, nj = jobs[q]
                if h < 0:
                    return swa2(q)
                hs = h % 2
                ci = chunk_idx[(h, c)]
                sl = ci % 2
                o = ci % 2
                b = q % 3
                self.act(pt[:, b, :], ps[b][:], AF.Exp, [Rps[b], Rbt[sl]], [Rpt[b]], bias=bt[:, sl, j:j + 1])
                self.mm(po[o][0:65, :], vca[:, hs, j, 0:65], pt[:, b, :], j == 0, j == nj - 1, [Rvca[hs], Rpt[b]], [Rpo[o]])

            def stage3(q):
                h, c, j, nj = jobs[q]
                if h < 0:
                    return swa3(q)
                if j != nj - 1:
                    return
                ci = chunk_idx[(h, c)]
                sl = ci % 2
                o = ci % 2
                cols = slice(c * 512, (c + 1) * 512)
                self._normalize(po[o], Rpo[o], pb[o], Rpb[o], rd[:, o], Rrd[o], osb[:, o], Rosb[o],
                                yc[:, sl, :], Ryc[sl])
                self.dma('pool', self.YT[768 + h * 64:768 + (h + 1) * 64, cols], yc[:, sl, :], [Ryc[sl]], (), owner=Ryc[sl])
            if getattr(self, 'verbose', False):
                print("fox sbuf remaining", nc.sbuf_bytes_remaining)
            NJ = len(jobs)
            DEF = 2
            STEP = max(1, (NJ - 8) // (len(side) + 1))
            si = 0
            for q in range(NJ + LA + DEF):
                if 0 <= q - LA - DEF < NJ:
                    stage3(q - LA - DEF)
                if q < NJ:
                    stage1(q)
                if 0 <= q - LA < NJ:
                    stage2(q - LA)
                if q % STEP == STEP - 1 and si < len(side):
                    side[si](); si += 1
            while si < len(side):
                side[si](); si += 1
            exh, Rexh = self.lru_exh
            self.dma('sp', self.EXH, exh[:], [Rexh], (), owner=Rexh)
            self.allgather(self.EXH, self.EXHg, "cc_h")
            s.barrier()
            s.flush()

    def phase_moe(self, l, last):
        nc, s = self.nc, self.s
        T, NCH, NT = self.T, self.NCH, self.NT
        RC, RM = self.R_const, self.R_mod
        xsrc = self.x_in if l == 0 else self.xs
        with ExitStack() as ph:
            def A(name, shape, dtype):
                return ph.enter_context(nc.sbuf_tensor(self.un(name), list(shape), dtype))

            def P(name, shape, dtype):
                return ph.enter_context(nc.psum_tensor(self.un(name), list(shape), dtype))
            woutb = A("woutb", [128, 8, D], BF16); Rwo = s.res("woutb")
            self.dma('sp', woutb[:], self.WO, (), [Rwo], owner=Rwo)
            wr = A("wr", [128, 8, 20], F32); brb = A("brb", [128, 20], F32); Rwr = s.res("wr")
            self.dma('sp', wr[:, :, 0:4], self.w_rg[l].rearrange("(kc p) n -> p kc n", p=128), (), [Rwr], owner=Rwr)
            self.dma('sp', wr[:, :, 4:20], self.w_re[l].rearrange("(kc p) n -> p kc n", p=128), (), [Rwr], owner=Rwr)
            self.dma('sp', brb[:, 0:4], self.b_rg[l].partition_broadcast(128), (), [Rwr], owner=Rwr)
            self.dma('sp', brb[:, 4:20], self.b_re[l].partition_broadcast(128), (), [Rwr], owner=Rwr)
            invw = A("invw", [128, 3], F32)
            self.memset('pool', invw[:, 0:1], 1.0 / 512, [Rwr])
            self.memset('pool', invw[:, 1:3], 1.0 / 256, [Rwr])
            sel = A("sel", [16, 16, 128], BF16)
            self.cp('dve', sel[:], self.ident_f[0:16, 0:16].unsqueeze(2).to_broadcast([16, 16, 128]), [RC], [Rwr])
            h0 = A("h0", [128, 2], F32); Rh0 = s.res("h0")
            self.dma('sp', self.shm[:], self.MODS[l, :, 5 * D:6 * D], (), [RM], owner=RM)
            self.dma('sp', h0[:], self.EXHg[0:128, 0:2], (), [Rh0], owner=Rh0)
            self.ts('dve', h0[:], h0[:], self.flg[:, 0:1], None, ALU.mult, None, [Rh0, RC], [Rh0])
            yT = A("yT", [128, 8, 512], BF16); RyT = s.res("yT")
            zt = A("zt", [128, 2, 512], BF16); Rzt = s.res("zt")
            sqy = A("sqy", [128, 2, 8, 128], BF16); Rsqy = [s.res("sqy0"), s.res("sqy1")]
            x1 = A("x1", [128, 2, 4, D], F32); Rx1 = [s.res("x10"), s.res("x11")]
            rs = A("rs", [128, 2, 12], F32); Rrs = [s.res("rs0"), s.res("rs1")]
            junk = A("junk", [128, D], BF16); Rjunk = s.res("junk")
            st = A("st", [128, 2, 4], F32); Rst = [s.res("st0"), s.res("st1")]
            u2 = A("u2", [128, 1, D], F32); Ru2 = [s.res("u20")] * 2
            h2 = A("h2", [128, 1, D], F32); Rh2 = [s.res("h20")] * 2
            h2T32 = A("h2T32", [128, 1, 8, 128], F32); Rh32 = [s.res("h2T320")] * 2
            h2T = A("h2T", [128, 2, 8, 512], BF16); Rh2T = [s.res("h2T0"), s.res("h2T1")]
            rtt = A("rtr", [128, 2, 96], F32); Rrtt = [s.res("rtr0"), s.res("rtr1")]
            combT = A("combT", [16, 2, 512], BF16); RcT = [s.res("combT0"), s.res("combT1")]
            wgu = A("wgu", [128, 3, 8, 512], BF16); Rwgu = [s.res(f"wgu{i}") for i in range(3)]
            t1 = A("t1", [128, 2, 512], F32); Rt1 = [s.res("t10"), s.res("t11")]
            t2 = A("t2", [128, 2, 512], F32); Rt2 = [s.res("t20"), s.res("t21")]
            hid = A("hid", [128, 32, 512], BF16); Rhid = s.res("hid")
            wd = A("wd", [128, 2, 8, 512], BF16); Rwd = [s.res("wd0"), s.res("wd1")]
            B = [P(f"B{i}", [128, 512], F32) for i in range(4)]; RB = [s.res(f"B{i}") for i in range(4)]
            C0 = P("C0", [128, 512], F32); RC0 = s.res("C0")
            F01 = P("F01", [128, 8, 128], F32); RF01 = s.res("F01")
            F2 = P("F2", [128, 512], F32); RF2 = s.res("F2")
            F01f = F01[:].rearrange("p a b -> p (a b)")
            GR = [(0, [0, 1, 2, 3]), (1, [4, 5]), (2, [6, 7])]
            WB = [(F01f[:, 0:512], RF01), (F01f[:, 512:1024], RF01), (F2[:], RF2)]

            def front_pieces(c):
                sl = c % 2
                cols = slice(c * 512, (c + 1) * 512)
                X = x1[:, sl]
                RX = Rx1[sl]
                pcs = []

                def p_load():
                    self.dma('sp', yT[:], self.YT[:, cols].rearrange("(fc p) t -> p fc t", p=128), (), [RyT], owner=RyT)
                    self.dma('sp', zt[:], self.ZT[:, cols].rearrange("(k p) t -> p k t", p=128), (), [Rzt], owner=Rzt)
                    self.dma('sp', X, xsrc[c * 512:(c + 1) * 512, :].rearrange("(i p) d -> p i d", p=128), (), [RX], owner=RX)
                pcs.append(p_load)

                def p_corr():
                    for k in range(2):
                        self.stt(yT[:, 4 + k, :], zt[:, k, :], h0[:, k:k + 1], yT[:, 4 + k, :], ALU.mult, ALU.add, [Rzt, Rh0, RyT], [RyT])
                pcs.append(p_corr)

                def p_sq(i):
                    self.act(sqy[:, i % 2], yT[:, :, i * 128:(i + 1) * 128], AF.Square, [RyT], [Rsqy[i % 2]])

                def p_ss(i):
                    for g, fcs in GR:
                        for k_, fc in enumerate(fcs):
                            self.mm(F2[:, 256 + i * 3 + g:256 + i * 3 + g + 1], sqy[:, i % 2, fc, :], self.ones_b[:, 0:1],
                                    k_ == 0, k_ == len(fcs) - 1, [Rsqy[i % 2], RC], [RF2])
                pcs.append(lambda: p_sq(0))
                for i in range(4):
                    pcs.append(lambda i=i: (p_ss(i), p_sq(i + 1) if i < 3 else None))

                def p_rs():
                    r = rs[:, sl]
                    self.tt('dve', r.rearrange("p (a b) -> p a b", a=4), F2[:, 256:268].rearrange("p (a b) -> p a b", a=4),
                            invw[:].unsqueeze(1).to_broadcast([128, 4, 3]), ALU.mult, [RF2, Rwr], [Rrs[sl]])
                    self.ts('dve', r, r, EPS, None, ALU.add, None, [Rrs[sl]], [Rrs[sl]])
                    self.act(r, r, AF.Sqrt, [Rrs[sl]], [Rrs[sl]])
                    self.recip(r, r, [Rrs[sl]], [Rrs[sl]])
                pcs.append(p_rs)

                def p_wout(i, nh):
                    for g, fcs in GR:
                        bank, Rb = WB[g]
                        for k_, fc in enumerate(fcs):
                            self.mm(bank, yT[:, fc, i * 128:(i + 1) * 128], woutb[:, fc, nh * 512:(nh + 1) * 512],
                                    k_ == 0, k_ == len(fcs) - 1, [RyT, Rwo], [Rb])
                    for g, _ in GR:
                        bank, Rb = WB[g]
                        xs_ = X[:, i, nh * 512:(nh + 1) * 512]
                        self.stt(xs_, bank, rs[:, sl, i * 3 + g:i * 3 + g + 1], xs_, ALU.mult, ALU.add, [Rb, Rrs[sl], RX], [RX])
                for i in range(4):
                    for nh in range(2):
                        pcs.append(lambda i=i, nh=nh: p_wout(i, nh))

                def p_norm(i):
                    q = i % 2
                    self.act(junk[:], X[:, i, :], AF.Square, [RX], [Rjunk, Rst[q]], accum_out=st[:, q, 0:1])
                    self.ts('dve', st[:, q, 1:2], st[:, q, 0:1], 1.0 / D, EPS, ALU.mult, ALU.add, [Rst[q]], [Rst[q]])
                    self.act(st[:, q, 2:3], st[:, q, 1:2], AF.Sqrt, [Rst[q]], [Rst[q]])
                    self.recip(st[:, q, 3:4], st[:, q, 2:3], [Rst[q]], [Rst[q]])
                    self.stt(u2[:, 0], X[:, i, :], st[:, q, 3:4], self.A_ffn[:], ALU.mult, ALU.mult, [RX, Rst[q], RM], [Ru2[q]])
                    self.tt('pool', h2[:, 0], u2[:, 0], self.shf[:], ALU.add, [Ru2[q], RM], [Rh2[q]])

                def p_tr(i):
                    q = i % 2
                    for dc in range(8):
                        self.tr(F01[:, dc, :], h2[:, 0, dc * 128:(dc + 1) * 128], self.ident_f[:], [Rh2[q], RC], [RF01])
                    self.cp('act', h2T32[:, 0], F01[:], [RF01], [Rh32[q]])
                    self.cp('pool', h2T[:, sl, :, i * 128:(i + 1) * 128], h2T32[:, 0], [Rh32[q]], [Rh2T[sl]])

                def p_route(i):
                    q = i % 2
                    rt = rtt[:, q]
                    for dc in range(8):
                        self.mm(F2[:, 0:20], h2T32[:, 0, dc, :], wr[:, dc, :], dc == 0, dc == 7, [Rh32[q], Rwr], [RF2])
                    lg = rt[:, 0:20]; gl = rt[:, 0:4]; el3 = rt[:, 4:20].rearrange("p (g i) -> p g i", g=4)
                    gmax = rt[:, 20:21]; ngmax = rt[:, 21:22]; goh = rt[:, 22:26]; ge = rt[:, 26:30]; gsum = rt[:, 30:31]
                    tmp3 = rt[:, 32:48].rearrange("p (g i) -> p g i", g=4); ing = rt[:, 48:52]
                    m1 = rt[:, 52:53]; nm1 = rt[:, 53:54]; oh1 = rt[:, 54:58]; msk = rt[:, 58:62]; m2 = rt[:, 62:63]
                    selm = rt[:, 64:68]; ee = rt[:, 68:72]; es = rt[:, 72:76]; ssum = rt[:, 76:77]; fac = rt[:, 77:78]
                    comb = rt[:, 80:96]
                    RR = [Rrtt[q]]
                    self.tt('dve', lg, F2[:, 0:20], brb[:], ALU.add, [RF2, Rwr], RR)
                    s.op('dve', lambda e: e.tensor_reduce(out=gmax, in_=gl, axis=AX.X, op=ALU.max), RR, RR)
                    self.ts('dve', goh, gl, gmax, None, ALU.is_equal, None, RR, RR)
                    self.ts('dve', ngmax, gmax, -1.0, None, ALU.mult, None, RR, RR)
                    self.act(ge, gl, AF.Exp, RR, RR, bias=ngmax, accum_out=gsum)
                    self.tt('dve', tmp3, el3, goh.unsqueeze(2).to_broadcast([128, 4, 4]), ALU.mult, RR, RR)
                    s.op('dve', lambda e: e.tensor_reduce(out=ing, in_=tmp3.rearrange("p g i -> p i g"), axis=AX.X, op=ALU.add), RR, RR)
                    s.op('dve', lambda e: e.tensor_reduce(out=m1, in_=ing, axis=AX.X, op=ALU.max), RR, RR)
                    self.ts('dve', oh1, ing, m1, None, ALU.is_equal, None, RR, RR)
                    self.stt(msk, oh1, -1e30, ing, ALU.mult, ALU.add, RR, RR)
                    s.op('dve', lambda e: e.tensor_reduce(out=m2, in_=msk, axis=AX.X, op=ALU.max), RR, RR)
                    self.ts('dve', selm, ing, m2, None, ALU.is_ge, None, RR, RR)
                    self.ts('dve', nm1, m1, -1.0, None, ALU.mult, None, RR, RR)
                    self.act(ee, ing, AF.Exp, RR, RR, bias=nm1)
                    self.tt('dve', es, ee, selm, ALU.mult, RR, RR)
                    s.op('dve', lambda e: e.tensor_reduce(out=ssum, in_=es, axis=AX.X, op=ALU.add), RR, RR)
                    self.tt('dve', fac, ssum, gsum, ALU.mult, RR, RR)
                    self.recip(fac, fac, RR, RR)
                    self.ts('dve', es, es, fac, None, ALU.mult, None, RR, RR)
                    self.tt('dve', comb.rearrange("p (g i) -> p g i", g=4), goh.unsqueeze(2).to_broadcast([128, 4, 4]),
                            es.unsqueeze(1).to_broadcast([128, 4, 4]), ALU.mult, RR, RR)

                def p_comb(i):
                    q = i % 2
                    self.tr(F2[0:16, 128:256], rtt[:, q, 80:96], self.ident_f[:], [Rrtt[q], RC], [RF2])
                    self.cp('dve', combT[:, sl, i * 128:(i + 1) * 128], F2[0:16, 128:256], [RF2], [RcT[sl]])
                for step in range(7):
                    def piece(step=step):
                        if 0 <= step - 3 < 4:
                            p_comb(step - 3)
                        if 0 <= step - 2 < 4:
                            p_route(step - 2)
                        if 0 <= step - 1 < 4:
                            p_tr(step - 1)
                        if step < 4:
                            p_norm(step)
                    pcs.append(piece)
                return pcs

            for p in front_pieces(0):
                p()
            ge_ = 0
            gw = 0

            def load_wgu(n):
                e = n % 16
                sl_ = n % 3
                self.dma('sp', wgu[:, sl_], self.WGU[e], (), [Rwgu[sl_]], owner=Rwgu[sl_])

            def load_wd(n):
                g_ = n % 8
                ws = n % 2
                self.dma('sp', wd[:, ws], self.WD[g_ // 4, g_ % 4], (), [Rwd[ws]], owner=Rwd[ws])
            tot_e = 16 * NCH
            tot_w = 8 * NCH
            for n in range(3):
                load_wgu(n)
            for c in range(NCH):
                sl = c % 2
                X = x1[:, sl]
                RX = Rx1[sl]
                pcs = front_pieces(c + 1) if c + 1 < NCH else []
                NSLOT = 24
                pi = 0

                def emit_pieces(slot):
                    nonlocal pi
                    tgt = (len(pcs) * (slot + 1)) // NSLOT
                    while pi < tgt:
                        pcs[pi]()
                        pi += 1
                for e in range(16):
                    n = c * 16 + e
                    ws_ = n % 3
                    for k in range(2):
                        pg, pu = (0, 1) if k == 0 else (2, 3)
                        for dc in range(8):
                            self.mm(B[pg][:], wgu[:, ws_, dc, k * 128:(k + 1) * 128], h2T[:, sl, dc, :], dc == 0, dc == 7,
                                    [Rwgu[ws_], Rh2T[sl]], [RB[pg]])
                        for dc in range(8):
                            self.mm(B[pu][:], wgu[:, ws_, dc, 256 + k * 128:256 + (k + 1) * 128], h2T[:, sl, dc, :], dc == 0, dc == 7,
                                    [Rwgu[ws_], Rh2T[sl]], [RB[pu]])
                        if k == 0:
                            self.mm(C0[:], sel[:, e, :], combT[:, sl, :], True, True, [Rwr, RcT[sl]], [RC0])
                        self.act(t1[:, k, :], B[pg][:], AF.Silu, [RB[pg]], [Rt1[k]])
                        self.tt('dve', t2[:, k, :], t1[:, k, :], B[pu][:], ALU.mult, [Rt1[k], RB[pu]], [Rt2[k]])
                        self.tt('dve', hid[:, e * 2 + k, :], t2[:, k, :], C0[:], ALU.mult, [Rt2[k], RC0], [Rhid])
                    if n + 3 < tot_e:
                        load_wgu(n + 3)
                    if e == 13:
                        load_wd(c * 8)
                    if e == 14:
                        load_wd(c * 8 + 1)
                    emit_pieces(e)
                for nh in range(2):
                    for eq in range(4):
                        n = c * 8 + nh * 4 + eq
                        ws = n % 2
                        for i in range(4):
                            for ek in range(8):
                                self.mm(B[i][:], hid[:, eq * 8 + ek, i * 128:(i + 1) * 128], wd[:, ws, ek, :],
                                        eq == 0 and ek == 0, eq == 3 and ek == 7, [Rhid, Rwd[ws]], [RB[i]])
                        if n + 2 < tot_w and not (nh == 1 and eq >= 2):
                            load_wd(n + 2)
                        emit_pieces(16 + nh * 4 + eq)
                    for i in range(4):
                        xs_ = X[:, i, nh * 512:(nh + 1) * 512]
                        self.tt('dve', t1[:, i % 2, :], B[i][:], self.shm[:, nh * 512:(nh + 1) * 512], ALU.mult, [RB[i], RM], [Rt1[i % 2]])
                        self.tt('dve', xs_, xs_, t1[:, i % 2, :], ALU.add, [RX, Rt1[i % 2]], [RX])
                if not last:
                    self.dma('pool', self.xs[c * 512:(c + 1) * 512, :].rearrange("(i p) d -> p i d", p=128), X, [RX], (), owner=RX)
                else:
                    for i in range(4):
                        q = i % 2
                        self.act(junk[:], X[:, i, :], AF.Square, [RX], [Rjunk, Rst[q]], accum_out=st[:, q, 0:1])
                        self.ts('dve', st[:, q, 1:2], st[:, q, 0:1], 1.0 / D, EPS, ALU.mult, ALU.add, [Rst[q]], [Rst[q]])
                        self.act(st[:, q, 2:3], st[:, q, 1:2], AF.Sqrt, [Rst[q]], [Rst[q]])
                        self.recip(st[:, q, 3:4], st[:, q, 2:3], [Rst[q]], [Rst[q]])
                        self.stt(X[:, i, :], X[:, i, :], st[:, q, 3:4], self.nfin[:], ALU.mult, ALU.mult, [RX, Rst[q], RC], [RX])
                    self.dma('pool', self.out[c * 512:(c + 1) * 512, :].rearrange("(i p) d -> p i d", p=128), X, [RX], (), owner=RX)
            s.barrier()
            s.flush()


def host_inputs(inputs, T=T_FULL):
    f = lambda a: np.ascontiguousarray(np.asarray(a, dtype=np.float32))
    L = L_FULL
    com = {
        "w_mod": f(inputs["w_mod"]), "b_mod": f(inputs["b_mod"]),
        "norm_mix": f(inputs["norm_mix"]), "norm_ffn": f(inputs["norm_ffn"]),
        "w_in": f(inputs["w_in"]), "w_out": f(inputs["w_out"]),
        "gainT": f(np.asarray(inputs["out_gain"]).reshape(L, 8, 128).transpose(0, 2, 1)),
        "sinks": f(inputs["sinks"]),
        "convT": f(np.asarray(inputs["conv_w"]).transpose(0, 2, 1).reshape(L, 2, 128, 4).transpose(0, 2, 1, 3)),
        "convb": f(np.asarray(inputs["conv_b"]).reshape(L, 2, 128).transpose(0, 2, 1)),
        "lru_wa": f(inputs["lru_wa"]), "lru_wx": f(inputs["lru_wx"]),
        "lru_ba": f(np.asarray(inputs["lru_ba"]).reshape(L, 2, 128).transpose(0, 2, 1)),
        "lru_bx": f(np.asarray(inputs["lru_bx"]).reshape(L, 2, 128).transpose(0, 2, 1)),
        "lru_lam": f(np.asarray(inputs["lru_lam"]).reshape(L, 2, 128).transpose(0, 2, 1)),
        "fox_bf": f(inputs["fox_bf"]),
        "w_rg": f(inputs["w_router_group"]), "b_rg": f(inputs["b_router_group"]),
        "w_re": f(inputs["w_router_expert"]), "b_re": f(inputs["b_router_expert"]),
        "w_gate": f(inputs["w_gate"]), "w_up": f(inputs["w_up"]), "w_down": f(inputs["w_down"]),
        "norm_final": f(inputs["norm_final"]),
    }
    x = np.asarray(inputs["x"], dtype=np.float32)
    c = np.asarray(inputs["c"], dtype=np.float32)
    maps = []
    for core in range(8):
        b, r = core // 2, core % 2
        m = dict(com)
        m["x"] = np.ascontiguousarray(x[b, r * T:(r + 1) * T])
        m["cT"] = np.ascontiguousarray(c[b].reshape(8, 128).T)
        fl = np.zeros((128, 4), np.float32)
        fl[:, 0] = float(r)
        fl[:, 1] = 0.0 if r else NEG
        m["flags"] = fl
        maps.append(m)
    return maps


_CACHE = {}


def kernel(**inputs):
    if "nc" not in _CACHE:
        _CACHE["nc"] = K().build()
    nc = _CACHE["nc"]
    maps = host_inputs(inputs)
    res = run_bass_kernel_spmd(nc, maps, core_ids=list(range(8)))
    out = np.stack([np.concatenate([np.asarray(res.results[2 * b + r]["out"], dtype=np.float32) for r in range(2)], axis=0)
                    for b in range(4)], axis=0)
    return out
```
